# Optimizing a Trainium2 kernel written in Bass

```python
import jax, jax.numpy as jnp
from jax import lax
import numpy as np

D_MODEL = 1024
BATCH = 4
SEQ = 8192
DEPTH = 1

HEAD_DIM = 64
N_SB_HEADS = 8
N_SW_HEADS = 8
N_SW_KV_HEADS = 2
D_SB = N_SB_HEADS * HEAD_DIM
D_SW = N_SW_HEADS * HEAD_DIM
D_SW_KV = N_SW_KV_HEADS * HEAD_DIM
D_MIX = D_SB + D_SW
D_IN = 3 * D_SB + D_SW + 2 * D_SW_KV
IN_SPLITS = (D_SB, 2 * D_SB, 3 * D_SB, 3 * D_SB + D_SW, 3 * D_SB + D_SW + D_SW_KV)
Q_BLOCK = 128
WINDOW = 128
ROPE_THETA = 10000.0
N_GROUPS = 4
EXPERTS_PER_GROUP = 4
N_EXPERTS = N_GROUPS * EXPERTS_PER_GROUP
TOP_K = 2
D_EXPERT = 512
EXPERT_BLOCK = 128
EPS = 1e-6

kernel_name = "hymba_sb_swa_hmoe_adaln_block"


def rmsnorm(x, g):
    xf = x.astype(jnp.float32)
    y = xf * lax.rsqrt(jnp.mean(xf * xf, axis=-1, keepdims=True) + EPS)
    return (y * g.astype(jnp.float32)).astype(x.dtype)


def rope(x, positions):
    d = x.shape[-1]
    inv_freq = ROPE_THETA ** (-jnp.arange(0, d, 2, dtype=jnp.float32) / d)
    ang = positions.astype(jnp.float32)[..., None] * inv_freq
    cos = jnp.cos(ang)[:, :, None, :]
    sin = jnp.sin(ang)[:, :, None, :]
    xf = x.astype(jnp.float32)
    x1, x2 = xf[..., : d // 2], xf[..., d // 2:]
    return jnp.concatenate([x1 * cos - x2 * sin, x2 * cos + x1 * sin], axis=-1).astype(x.dtype)


def stick_breaking_attention(q, k, v):
    B, S, H, d = q.shape
    nblk = S // Q_BLOCK
    scale = d ** -0.5
    qb = q.reshape(B, nblk, Q_BLOCK, H, d).transpose(1, 0, 2, 3, 4)
    kf = k.astype(jnp.float32)
    vf = v.astype(jnp.float32)
    key_pos = jnp.arange(S)

    def one_block(args):
        i, qi = args
        z = jnp.einsum('bqhd,bkhd->bhqk', qi.astype(jnp.float32), kf) * scale
        q_pos = i * Q_BLOCK + jnp.arange(Q_BLOCK)
        causal = key_pos[None, :] < q_pos[:, None]
        log_beta = jax.nn.log_sigmoid(z)
        log_one_minus = jnp.where(causal, jax.nn.log_sigmoid(-z), 0.0)
        suffix = lax.cumsum(log_one_minus, axis=3, reverse=True) - log_one_minus
        weights = jnp.where(causal, jnp.exp(log_beta + suffix), 0.0)
        return jnp.einsum('bhqk,bkhd->bqhd', weights, vf)

    out = lax.map(one_block, (jnp.arange(nblk), qb))
    return out.transpose(1, 0, 2, 3, 4).reshape(B, S, H, d).astype(q.dtype)


def sliding_window_attention(q, k, v, sinks):
    B, S, Hq, d = q.shape
    Hkv = k.shape[2]
    rep = Hq // Hkv
    nblk = S // WINDOW
    scale = d ** -0.5
    qb = q.reshape(B, nblk, WINDOW, Hkv, rep, d)

    def with_prev(t):
        tb = t.reshape(B, nblk, WINDOW, Hkv, d)
        prev = jnp.pad(tb, ((0, 0), (1, 0), (0, 0), (0, 0), (0, 0)))[:, :-1]
        return jnp.concatenate([prev, tb], axis=2)

    kb = with_prev(k)
    vb = with_prev(v)
    s = jnp.einsum('bnqgrd,bnkgd->bngrqk', qb, kb,
                   preferred_element_type=jnp.float32) * scale
    qi = jnp.arange(WINDOW)[:, None]
    kk = jnp.arange(2 * WINDOW)[None, :]
    diff = WINDOW + qi - kk
    in_window = (diff >= 0) & (diff < WINDOW)
    blk = jnp.arange(nblk)[:, None, None]
    key_exists = blk * WINDOW + kk[None] - WINDOW >= 0
    mask = (in_window[None] & key_exists)[None, :, None, None]
    s = jnp.where(mask, s, -1e30)
    sink = sinks.astype(jnp.float32).reshape(1, 1, Hkv, rep, 1, 1)
    m = jnp.maximum(jnp.max(s, axis=-1, keepdims=True), sink)
    p = jnp.exp(s - m)
    denom = jnp.sum(p, axis=-1, keepdims=True) + jnp.exp(sink - m)
    p = (p / denom).astype(v.dtype)
    out = jnp.einsum('bngrqk,bnkgd->bnqgrd', p, vb)
    return out.reshape(B, S, Hq, d)


def hierarchical_moe(h, w_router_group, w_router_expert, w_gate, w_up, w_down):
    B, S, D = h.shape
    T = B * S
    xt = h.reshape(T, D)
    group_logits = jnp.einsum('td,dg->tg', xt, w_router_group, preferred_element_type=jnp.float32)
    group_probs = jax.nn.softmax(group_logits, axis=-1)
    g_sel = jnp.argmax(group_logits, axis=-1)
    g_prob = jnp.take_along_axis(group_probs, g_sel[:, None], axis=-1)
    expert_logits = jnp.einsum('td,de->te', xt, w_router_expert,
                               preferred_element_type=jnp.float32).reshape(T, N_GROUPS, EXPERTS_PER_GROUP)
    within = jnp.take_along_axis(expert_logits, g_sel[:, None, None], axis=1)[:, 0]
    top_logits, top_local = lax.top_k(within, TOP_K)
    top_w = jax.nn.softmax(top_logits, axis=-1) * g_prob
    expert_id = g_sel[:, None] * EXPERTS_PER_GROUP + top_local

    A = T * TOP_K
    flat_e = expert_id.reshape(A).astype(jnp.int32)
    flat_w = top_w.reshape(A)
    flat_tok = jnp.arange(A, dtype=jnp.int32) // TOP_K
    order = jnp.argsort(flat_e)
    sorted_e = flat_e[order]
    counts = jax.ops.segment_sum(jnp.ones_like(flat_e), flat_e, num_segments=N_EXPERTS)
    padded = (counts + EXPERT_BLOCK - 1) // EXPERT_BLOCK * EXPERT_BLOCK
    start = jnp.cumsum(counts) - counts
    pstart = jnp.cumsum(padded) - padded
    dest = pstart[sorted_e] + (jnp.arange(A, dtype=jnp.int32) - start[sorted_e])
    P = A + N_EXPERTS * EXPERT_BLOCK
    nb = P // EXPERT_BLOCK
    buf_tok = jnp.zeros((P,), jnp.int32).at[dest].set(flat_tok[order])
    buf_w = jnp.zeros((P,), jnp.float32).at[dest].set(flat_w[order])
    block_start = jnp.arange(nb, dtype=jnp.int32) * EXPERT_BLOCK
    block_e = jnp.minimum(jnp.searchsorted(pstart + padded, block_start, side='right'),
                          N_EXPERTS - 1).astype(jnp.int32)

    def expert_block(args):
        tok, e = args
        xb = xt[tok]
        hidden = jax.nn.silu(xb @ w_gate[e]) * (xb @ w_up[e])
        return hidden @ w_down[e]

    yb = lax.map(expert_block, (buf_tok.reshape(nb, EXPERT_BLOCK), block_e))
    y = jax.ops.segment_sum(yb.reshape(P, D) * buf_w[:, None].astype(yb.dtype), buf_tok,
                            num_segments=T)
    return y.reshape(B, S, D).astype(h.dtype)


def setup_inputs(seed: int = 0) -> dict:
    key = jax.random.key(seed)
    ks = jax.random.split(key, 20)

    def nrm(k, shape, s):
        return jax.random.normal(k, shape, jnp.float32) * s

    x = nrm(ks[0], (BATCH, SEQ, D_MODEL), 1.0)
    c = nrm(ks[1], (BATCH, D_MODEL), 1.0)
    offsets = jax.random.randint(ks[2], (BATCH, 1), 0, 4096, dtype=jnp.int32)
    positions = offsets + jnp.arange(SEQ, dtype=jnp.int32)[None, :]
    return {
        "x": x,
        "c": c,
        "positions": positions,
        "w_ada": nrm(ks[3], (DEPTH, D_MODEL, 6 * D_MODEL), D_MODEL ** -0.5),
        "b_ada": nrm(ks[4], (DEPTH, 6 * D_MODEL), 0.01),
        "norm_mix_g": 1.0 + nrm(ks[5], (DEPTH, D_MODEL), 0.02),
        "w_in": nrm(ks[6], (DEPTH, D_MODEL, D_IN), D_MODEL ** -0.5),
        "sinks": nrm(ks[7], (DEPTH, N_SW_HEADS), 1.0),
        "out_norm_sb_g": 1.0 + nrm(ks[8], (DEPTH, D_SB), 0.02),
        "out_norm_sw_g": 1.0 + nrm(ks[9], (DEPTH, D_SW), 0.02),
        "w_out": nrm(ks[10], (DEPTH, D_MIX, D_MODEL), D_MIX ** -0.5),
        "norm_ffn_g": 1.0 + nrm(ks[11], (DEPTH, D_MODEL), 0.02),
        "w_router_group": nrm(ks[12], (DEPTH, D_MODEL, N_GROUPS), D_MODEL ** -0.5),
        "w_router_expert": nrm(ks[13], (DEPTH, D_MODEL, N_EXPERTS), D_MODEL ** -0.5),
        "w_gate": nrm(ks[14], (DEPTH, N_EXPERTS, D_MODEL, D_EXPERT), D_MODEL ** -0.5),
        "w_up": nrm(ks[15], (DEPTH, N_EXPERTS, D_MODEL, D_EXPERT), D_MODEL ** -0.5),
        "w_down": nrm(ks[16], (DEPTH, N_EXPERTS, D_EXPERT, D_MODEL), D_EXPERT ** -0.5),
        "norm_final_g": 1.0 + nrm(ks[17], (D_MODEL,), 0.02),
    }


def reference(x, c, positions, w_ada, b_ada, norm_mix_g, w_in, sinks, out_norm_sb_g, out_norm_sw_g,
              w_out, norm_ffn_g, w_router_group, w_router_expert, w_gate, w_up, w_down, norm_final_g):
    B, S, D = x.shape
    for l in range(DEPTH):
        mod = jax.nn.silu(c) @ w_ada[l] + b_ada[l]
        shift1, scale1, gate1, shift2, scale2, gate2 = jnp.split(mod[:, None, :], 6, axis=-1)

        h = rmsnorm(x, norm_mix_g[l]) * (1.0 + scale1) + shift1
        proj = jnp.einsum('bsd,de->bse', h, w_in[l])
        q_sb, k_sb, v_sb, q_sw, k_sw, v_sw = jnp.split(proj, IN_SPLITS, axis=-1)
        o_sb = stick_breaking_attention(q_sb.reshape(B, S, N_SB_HEADS, HEAD_DIM),
                                        k_sb.reshape(B, S, N_SB_HEADS, HEAD_DIM),
                                        v_sb.reshape(B, S, N_SB_HEADS, HEAD_DIM))
        q_sw = rope(q_sw.reshape(B, S, N_SW_HEADS, HEAD_DIM), positions)
        k_sw = rope(k_sw.reshape(B, S, N_SW_KV_HEADS, HEAD_DIM), positions)
        o_sw = sliding_window_attention(q_sw, k_sw, v_sw.reshape(B, S, N_SW_KV_HEADS, HEAD_DIM), sinks[l])
        mixed = jnp.concatenate([rmsnorm(o_sb.reshape(B, S, D_SB), out_norm_sb_g[l]),
                                 rmsnorm(o_sw.reshape(B, S, D_SW), out_norm_sw_g[l])], axis=-1)
        x = x + gate1 * jnp.einsum('bse,ed->bsd', mixed, w_out[l])

        h2 = rmsnorm(x, norm_ffn_g[l]) * (1.0 + scale2) + shift2
        x = x + gate2 * hierarchical_moe(h2, w_router_group[l], w_router_expert[l],
                                         w_gate[l], w_up[l], w_down[l])
    return rmsnorm(x, norm_final_g)
```

```python
import os
import contextlib
import numpy as np
import concourse.bass as bass
import concourse.mybir as mybir
from concourse.bass_utils import run_bass_kernel_spmd

F32 = mybir.dt.float32
BF16 = mybir.dt.bfloat16
I32 = mybir.dt.int32
AF = mybir.ActivationFunctionType
ALU = mybir.AluOpType
AX = mybir.AxisListType

S = 8192
D = 1024
NEXP = 16
NEG = -30000.0
BIG = 1.0e30
EPS = 1e-6
TWO_PI = 6.283185307179586
T_R = ([0, 3, 4, 7, 8, 11, 12, 15], [1, 2, 5, 6, 9, 10, 13, 14])
C_Q, C_K, C_V, C_QS, C_QSS, C_KS, C_KSS, C_VS, C_END = 0, 512, 1024, 1536, 2048, 2560, 2688, 2816, 2944
V_BADA, V_GMIX, V_GFFN, V_GO, V_GFIN, V_SINK, V_INVF, V_SGN, V_N = 0, 48, 56, 64, 72, 80, 88, 89, 90


class Buf:
    __slots__ = ("name", "writers", "readers", "pend")

    def __init__(self, name):
        self.name = name
        self.writers = {}
        self.readers = {}
        self.pend = {}


class Eng:
    def __init__(self, fw, name, is_pe=False, has_sem=True):
        self.fw = fw
        self.name = name
        self.is_pe = is_pe
        self.key = "e_" + name
        self.sem = fw.new_sem(self.key) if has_sem else None
        self.cnt = 0
        self.seen = {}
        self.prog = []
        self.n_instr = 0

    def _wait(self, semkey, val):
        if val <= 0 or self.seen.get(semkey, 0) >= val:
            return
        self.seen[semkey] = val
        sem = self.fw.sems[semkey]
        self.prog.append(lambda e, sem=sem, val=val: e.wait_ge(sem, val))

    def _deps(self, reads, writes, partial):
        for b in reads:
            for k, v in b.writers.items():
                if not (self.is_pe and k == self.key):
                    self._wait(k, v)
        for b in writes:
            for k, v in b.readers.items():
                if not (self.is_pe and k == self.key):
                    self._wait(k, v)
            if not partial:
                for k, v in b.writers.items():
                    if not (self.is_pe and k == self.key):
                        self._wait(k, v)
            else:
                for k, v in b.pend.items():
                    if not (self.is_pe and k == self.key):
                        self._wait(k, v)

    def _mark(self, key, val, reads, writes, partial):
        for b in writes:
            if not partial:
                pend = dict(b.readers)
                for k, v in b.writers.items():
                    pend[k] = max(pend.get(k, 0), v)
                b.pend = pend
                b.writers = {}
                b.readers = {}
            b.writers[key] = max(b.writers.get(key, 0), val)
        for b in reads:
            b.readers[key] = max(b.readers.get(key, 0), val)

    def op(self, fn, reads=(), writes=(), sig=True, partial=False):
        self._deps(reads, writes, partial)
        if sig:
            self.cnt += 1
            assert self.cnt < 60000, self.name
            val = self.cnt
            sem = self.sem
            self.prog.append(lambda e, fn=fn, sem=sem: fn(e).then_inc(sem, 1))
        else:
            val = self.cnt + 1
            self.prog.append(lambda e, fn=fn: fn(e))
        self.n_instr += 1
        self._mark(self.key, val, reads, writes, partial)

    def dma(self, out, in_, semkey, reads=(), writes=(), partial=False, **kw):
        self._deps(reads, writes, partial)
        fw = self.fw
        if semkey not in fw.sems:
            fw.new_sem(semkey)
        fw.semcnt[semkey] = fw.semcnt.get(semkey, 0) + 16
        val = fw.semcnt[semkey]
        assert val < 60000, semkey
        sem = fw.sems[semkey]
        self.prog.append(lambda e, out=out, in_=in_, sem=sem, kw=kw:
                         e.dma_start(out=out, in_=in_, **kw).then_inc(sem, 16))
        self.n_instr += 1
        self._mark(semkey, val, reads, writes, partial)

    def wait_all(self, bufs):
        for b in bufs:
            for k, v in list(b.writers.items()) + list(b.readers.items()):
                self._wait(k, v)


class FW:
    def __init__(self, nc, stack):
        self.nc = nc
        self.stack = stack
        self.sems = {}
        self.semcnt = {}
        self.pe = Eng(self, "pe", is_pe=True)
        self.act = Eng(self, "act")
        self.dve = Eng(self, "dve")
        self.pool = Eng(self, "pool")
        self.sp = Eng(self, "sp", has_sem=False)
        self.engs = [self.pe, self.act, self.dve, self.pool, self.sp]

    def new_sem(self, key):
        s = self.stack.enter_context(self.nc.semaphore(key))
        self.sems[key] = s
        return s

    def barrier(self):
        for e in self.engs:
            for q in (self.pe, self.act, self.dve, self.pool):
                if q is not e:
                    e._wait(q.key, q.cnt)
                elif not e.is_pe:
                    e._wait(q.key, q.cnt)
            for k, v in self.semcnt.items():
                e._wait(k, v)

    def finish(self):
        nc = self.nc
        with nc.Block() as block:
            @block.tensor
            def _(e):
                for t in self.pe.prog:
                    t(e)

            @block.scalar
            def _(e):
                for t in self.act.prog:
                    t(e)

            @block.vector
            def _(e):
                for t in self.dve.prog:
                    t(e)

            @block.gpsimd
            def _(e):
                for t in self.pool.prog:
                    t(e)

            @block.sync
            def _(e):
                for t in self.sp.prog:
                    t(e)


class Arena:
    def __init__(self, nc, nbytes):
        self.nbytes = nbytes
        self.f = nc.alloc_sbuf_tensor("arena", [128, nbytes // 4], F32)
        self.b = self.f.bitcast(BF16)
        self.i = self.f.bitcast(I32)
        self.top = 0

    def reset(self, off):
        self.top = off

    def get(self, dt, shape):
        es = 2 if dt == BF16 else 4
        n = 1
        for s_ in shape[1:]:
            n *= s_
        nb = (n * es + 31) // 32 * 32
        off = self.top
        self.top += nb
        assert self.top <= self.nbytes, ("arena overflow", self.top, self.nbytes)
        h = self.b if dt == BF16 else (self.i if dt == I32 else self.f)
        ap = h[0:shape[0], off // es: off // es + n]
        if len(shape) == 3:
            ap = ap.rearrange("p (a b) -> p a b", b=shape[2])
        return ap


def build_program(debug_outs=(), stop_after=99):
    nc = bass.Bass("TRN2", target_bir_lowering=False)
    dbg = set(debug_outs)

    def din(name, shape, dt=F32):
        return nc.dram_tensor(name, list(shape), dt, kind="ExternalInput").ap()

    def dscr(name, shape, dt):
        kind = "ExternalOutput" if name in dbg else "Internal"
        return nc.dram_tensor(name, list(shape), dt, kind=kind).ap()

    xs = din("xs", [S, D])
    xo = din("xo", [8, 640, D])
    posb = din("posb", [8, 128, 640], I32)
    cT = din("cT", [128, 8])
    w_ada = din("w_ada", [D, 6 * D])
    vecs = din("vecs", [128, V_N])
    w_inr = din("w_inr", [D, C_END])
    w_out = din("w_out", [D, D])
    w_r = din("w_r", [D, 20])
    w_gate = din("w_gate", [NEXP, D, 512])
    w_up = din("w_up", [NEXP, D, 512])
    w_down = din("w_down", [NEXP, 512, D])
    cmat = din("cmat", [4, 128, 128])
    amask = din("amask", [16, 128, 512])
    swb = din("swb", [2, 128, 512])
    out = nc.dram_tensor("out", [4096, D], F32, kind="ExternalOutput").ap()

    KT_s = dscr("KT_s", [4, 128, S], BF16)
    V_s = dscr("V_s", [S, 512], BF16)
    QT_s = dscr("QT_s", [4, 128, 4096], BF16)
    O_s = dscr("O_s", [4096, D], BF16)
    X1_s = dscr("X1_s", [4096, D], F32)
    H2T_s = dscr("H2T_s", [8, 128, 4096], BF16)
    MOD_s = dscr("MOD_s", [128, 48], F32)
    GW_s = dscr("GW_s", [128, 32 * 16], F32)

    with contextlib.ExitStack() as st:
        fw = FW(nc, st)
        pe, act, dve, pool, sp = fw.pe, fw.act, fw.dve, fw.pool, fw.sp
        A = Arena(nc, 211968)
        PSP = [nc.alloc_psum_tensor("psp%d" % i, [128, 1024], F32) for i in range(4)]
        PSPB = [p.bitcast(BF16) for p in PSP]
        PS = [PSP[i // 2][:, (i % 2) * 512:(i % 2 + 1) * 512] for i in range(8)]
        PSB = [PSPB[i // 2][:, (i % 2) * 1024:(i % 2 + 1) * 1024] for i in range(8)]
        bPS = [Buf("ps%d" % i) for i in range(8)]

        def ACT(out_, in_, func, reads, writes, bias=None, scale=None, accum=None, partial=False):
            kw = {}
            if bias is not None:
                kw["bias"] = bias
            if scale is not None:
                kw["scale"] = scale
            if accum is not None:
                kw["accum_out"] = accum
            act.op(lambda e: e.activation(out=out_, in_=in_, func=func, **kw), reads, writes, partial=partial)

        def TS(out_, in0, s1, s2, op0, op1, reads, writes, partial=False, eng=None):
            q = eng or dve
            if op1 is None:
                q.op(lambda e: e.tensor_scalar(out=out_, in0=in0, scalar1=s1, scalar2=None, op0=op0), reads, writes, partial=partial)
            else:
                q.op(lambda e: e.tensor_scalar(out=out_, in0=in0, scalar1=s1, scalar2=s2, op0=op0, op1=op1), reads, writes, partial=partial)

        def TT(out_, in0, in1, op, reads, writes, partial=False, eng=None):
            q = eng or dve
            q.op(lambda e: e.tensor_tensor(out=out_, in0=in0, in1=in1, op=op), reads, writes, partial=partial)

        def STT(out_, in0, scalar, in1, op0, op1, reads, writes, partial=False):
            dve.op(lambda e: e.scalar_tensor_tensor(out=out_, in0=in0, scalar=scalar, in1=in1, op0=op0, op1=op1), reads, writes, partial=partial)

        def CP(out_, in_, reads, writes, partial=False, eng=None):
            q = eng or dve
            q.op(lambda e: e.tensor_copy(out=out_, in_=in_), reads, writes, partial=partial)

        def MM(out_, lhsT, rhs, start, stop, reads, writes, sig=False, skip=False):
            if skip:
                pe.op(lambda e: e.matmul(out_, lhsT=lhsT, rhs=rhs, start=start, stop=stop, skip_group_check=True), reads, writes, sig=sig, partial=True)
            else:
                pe.op(lambda e: e.matmul(out_, lhsT=lhsT, rhs=rhs, start=start, stop=stop), reads, writes, sig=sig, partial=True)

        def TR(out_, in_, ident, reads, writes, sig=False):
            pe.op(lambda e: e.transpose(out_, in_, ident), reads, writes, sig=sig, partial=True)

        def rstd_from_ssq(dst, ssq, n, reads, writes):
            ACT(dst, ssq, AF.Ln, reads, writes, bias=epsc, scale=1.0 / n)
            ACT(dst, dst, AF.Exp, writes, writes, scale=-0.5)

        identf = A.get(F32, [128, 128])
        onesf = A.get(F32, [128, 128])
        identb = A.get(BF16, [128, 128])
        trin = A.get(BF16, [128, 128])
        onesn = A.get(BF16, [128, 128])
        vec = A.get(F32, [128, V_N])
        modT = A.get(F32, [128, 48])
        gm1 = A.get(F32, [128, 8])
        gm2 = A.get(F32, [128, 8])
        epsc = A.get(F32, [128, 1])
        gate1_t = A.get(F32, [128, D])
        gate2_t = A.get(F32, [128, D])
        gfin_t = A.get(F32, [128, D])
        G_o = A.get(BF16, [128, 8, 128])
        GW = A.get(F32, [128, 32, 16])
        PERSIST_END = A.top
        bconst = Buf("const")
        bvec = Buf("vec")
        bmod = Buf("mod")
        bgates = Buf("gates")
        bGW = Buf("GW")
        bKT, bV, bQT, bO, bX1, bH2T = Buf("KT_s"), Buf("V_s"), Buf("QT_s"), Buf("O_s"), Buf("X1_s"), Buf("H2T_s")
        bOUT = Buf("out")
        bDBG = Buf("dbg")

        sh1 = modT[:, 0:8]
        sh2 = modT[:, 24:32]

        sp.dma(identf, cmat[0], "ld_cf", writes=[bconst])
        sp.dma(onesf, cmat[3], "ld_cf", writes=[bconst], partial=True)
        pool.dma(identb, cmat[0], "ld_cb", writes=[bconst], partial=True)
        pool.dma(trin, cmat[1], "ld_cb", writes=[bconst], partial=True)
        pool.dma(onesn, cmat[2], "ld_cb", writes=[bconst], partial=True)
        sp.dma(vec, vecs, "ld_vec", writes=[bvec])
        dve.op(lambda e: e.memset(epsc, EPS), (), [bconst], partial=True)

        A.reset(PERSIST_END)
        sc_in = A.get(F32, [128, 8])
        sc = A.get(F32, [128, 8])
        wa = [A.get(F32, [128, 8, 512]) for _ in range(2)]
        diag = [A.get(F32, [128, 128]) for _ in range(2)]
        bsc, bwa, bdiag = Buf("sc"), [Buf("wa0"), Buf("wa1")], [Buf("dg0"), Buf("dg1")]
        sp.dma(sc_in, cT, "ld_sc", writes=[bsc])
        ACT(sc, sc_in, AF.Silu, [bsc], [bsc])
        w_ada_v = w_ada.rearrange("(k p) n -> p k n", p=128)
        for nt in range(12):
            s_ = nt % 2
            sp.dma(wa[s_], w_ada_v[:, :, nt * 512:(nt + 1) * 512], "ld_wa%d" % s_, writes=[bwa[s_]])
            for m in range(4):
                j = 4 * nt + m
                for k in range(8):
                    MM(PS[0][:, j:j + 1], wa[s_][:, k, m * 128:(m + 1) * 128], sc[:, k:k + 1], k == 0, k == 7,
                       [bwa[s_], bsc], [bPS[0]], sig=(k == 7))
        TT(modT, PS[0][:, 0:48], vec[:, V_BADA:V_BADA + 48], ALU.add, [bPS[0], bvec], [bmod])
        STT(gm1, modT[:, 8:16], 1.0, vec[:, V_GMIX:V_GMIX + 8], ALU.add, ALU.mult, [bmod, bvec], [bmod], partial=True)
        STT(gm2, modT[:, 32:40], 1.0, vec[:, V_GFFN:V_GFFN + 8], ALU.add, ALU.mult, [bmod, bvec], [bmod], partial=True)
        nd = 0
        for (dst, src) in ((gate1_t, modT[:, 16:24]), (gate2_t, modT[:, 40:48]), (gfin_t, vec[:, V_GFIN:V_GFIN + 8])):
            for half in range(2):
                bank = 1 + half
                for kk in range(4):
                    k = half * 4 + kk
                    d_ = nd % 2
                    nd += 1
                    TS(diag[d_], identf, src[:, k:k + 1], None, ALU.mult, None, [bconst, bmod, bvec], [bdiag[d_]])
                    MM(PS[bank][:, kk * 128:(kk + 1) * 128], onesf, diag[d_], True, True, [bconst, bdiag[d_]], [bPS[bank]], sig=True)
                CP(dst[:, half * 512:(half + 1) * 512], PS[bank][:, :], [bPS[bank]], [bgates], partial=True)
        for k in range(8):
            TS(G_o[:, k, :], onesf, vec[:, V_GO + k:V_GO + k + 1], None, ALU.mult, None, [bconst, bvec], [bgates], partial=True)
        if "MOD_s" in dbg:
            sp.dma(MOD_s, modT, "st_dbg", reads=[bmod], writes=[bDBG], partial=True)
        fw.barrier()

        if stop_after >= 1:
            A.reset(PERSIST_END)
            win = A.get(BF16, [128, 8, C_END])
            xt = [A.get(F32, [128, 5, D]) for _ in range(2)]
            xn = [A.get(BF16, [128, 5, D]) for _ in range(2)]
            hT = [A.get(BF16, [128, 8, 640]) for _ in range(2)]
            ssq = [A.get(F32, [128, 8]) for _ in range(3)]
            rstd = [A.get(F32, [128, 8]) for _ in range(3)]
            kst = [A.get(BF16, [128, 4, 512]) for _ in range(2)]
            vst = [A.get(BF16, [128, 4, 512]) for _ in range(2)]
            bwin = Buf("win")
            bxt, bxn, bhT = [Buf("xt0"), Buf("xt1")], [Buf("xn0"), Buf("xn1")], [Buf("hT0"), Buf("hT1")]
            bjunk, bssq = Buf("junk"), [Buf("ssq0"), Buf("ssq1"), Buf("ssq2")]
            bkst, bvst = [Buf("kst0"), Buf("kst1")], [Buf("vst0"), Buf("vst1")]
            for k in range(8):
                for (c0, c1) in ((0, 1024), (1024, 2048), (2048, C_END)):
                    pool.dma(win[:, k, c0:c1], w_inr[k * 128:(k + 1) * 128, c0:c1], "ld_win", writes=[bwin], partial=True)

            def nt_T1(nblk, s_, q3):
                X, XN = xt[s_], xn[s_]
                for a in range(nblk):
                    ACT(XN[:, a, :], X[:, a, :], AF.Square, [bxt[s_]], [bxn[s_], bssq[q3]], accum=ssq[q3][:, a:a + 1], partial=True)
                rstd_from_ssq(rstd[q3][:, 0:nblk], ssq[q3][:, 0:nblk], float(D), [bssq[q3]], [bssq[q3]])

            def nt_T2(nblk, s_, q3):
                X, XN = xt[s_], xn[s_]
                for a in range(nblk):
                    TS(XN[:, a, :], X[:, a, :], rstd[q3][:, a:a + 1], None, ALU.mult, None, [bxt[s_], bssq[q3]], [bxn[s_]], partial=(a > 0))

            def nt_T3(nblk, s_):
                XN = xn[s_]
                W = nblk * 128
                for k in range(8):
                    bank = k % 2
                    for a in range(nblk):
                        TR(PSB[bank][:, a * 128:(a + 1) * 128], XN[:, a, k * 128:(k + 1) * 128], identb, [bxn[s_], bconst], [bPS[bank]], sig=(a == nblk - 1))
                    if bank == 0:
                        ACT(hT[s_][:, k, 0:W], PSB[bank][:, 0:W], AF.Identity, [bPS[bank], bmod], [bhT[s_]], bias=sh1[:, k:k + 1], scale=gm1[:, k:k + 1], partial=(k > 0))
                    else:
                        TS(hT[s_][:, k, 0:W], PSB[bank][:, 0:W], gm1[:, k:k + 1], sh1[:, k:k + 1], ALU.mult, ALU.add, [bPS[bank], bmod], [bhT[s_]], partial=True)

            xs_v = xs.rearrange("(t a p) f -> t p a f", p=128, a=4)

            def p1a_B(i):
                s_ = i % 2
                H = hT[s_]
                for hp in range(4):
                    bank = 2 + hp % 2
                    for k in range(8):
                        MM(PS[bank][:, :], win[:, k, C_K + hp * 128:C_K + (hp + 1) * 128], H[:, k, 0:512], k == 0, k == 7,
                           [bwin, bhT[s_]], [bPS[bank]], sig=(k == 7))
                    if hp % 2 == 0:
                        ACT(kst[s_][:, hp, :], PS[bank][:, :], AF.Copy, [bPS[bank]], [bkst[s_]], partial=(hp > 0))
                    else:
                        CP(kst[s_][:, hp, :], PS[bank][:, :], [bPS[bank]], [bkst[s_]], partial=True)
                sp.dma(KT_s[:, :, i * 512:(i + 1) * 512].rearrange("h p t -> p h t"), kst[s_], "st_k%d" % s_, reads=[bkst[s_]], writes=[bKT], partial=True)
                for a in range(4):
                    bank = 4 + a % 2
                    for k in range(8):
                        MM(PS[bank][:, :], H[:, k, a * 128:(a + 1) * 128], win[:, k, C_V:C_V + 512], k == 0, k == 7,
                           [bwin, bhT[s_]], [bPS[bank]], sig=(k == 7))
                    if a % 2 == 0:
                        ACT(vst[s_][:, a, :], PS[bank][:, :], AF.Copy, [bPS[bank]], [bvst[s_]], partial=(a > 0))
                    else:
                        CP(vst[s_][:, a, :], PS[bank][:, :], [bPS[bank]], [bvst[s_]], partial=True)
                sp.dma(V_s[i * 512:(i + 1) * 512, :].rearrange("(a p) c -> p a c", p=128), vst[s_], "st_v%d" % s_, reads=[bvst[s_]], writes=[bV], partial=True)

            sp.dma(xt[0][:, 0:4, :], xs_v[0], "ld_x0", writes=[bxt[0]])
            sp.dma(xt[1][:, 0:4, :], xs_v[1], "ld_x1", writes=[bxt[1]])
            for it in range(16 + 3):
                if 0 <= it - 3 < 16:
                    p1a_B(it - 3)
                if 0 <= it - 2 < 16:
                    nt_T3(4, (it - 2) % 2)
                if 0 <= it - 1 < 16:
                    i_ = it - 1
                    nt_T2(4, i_ % 2, i_ % 3)
                    if i_ + 2 < 16:
                        sp.dma(xt[i_ % 2][:, 0:4, :], xs_v[i_ + 2], "ld_x%d" % (i_ % 2), writes=[bxt[i_ % 2]])
                if it < 16:
                    nt_T1(4, it % 2, it % 3)

            qst = kst
            bqst = bkst
            ost = vst
            bost = bvst
            pos_i = A.get(I32, [128, 640])
            ang = A.get(F32, [128, 640])
            tqs = [A.get(F32, [128, 640]) for _ in range(2)]
            ti = A.get(I32, [128, 640])
            cosT = [A.get(F32, [128, 640]) for _ in range(2)]
            sinT = [A.get(F32, [128, 640]) for _ in range(2)]
            t1 = A.get(F32, [128, 512])
            t2 = A.get(F32, [128, 512])
            ksT = A.get(BF16, [128, 640])
            qsT = A.get(BF16, [128, 4, 512])
            vsw = A.get(BF16, [128, 5, 128])
            smx = [A.get(F32, [128, 512]) for _ in range(2)]
            pex = [A.get(BF16, [128, 512]) for _ in range(2)]
            pT = [A.get(BF16, [128, 512]) for _ in range(2)]
            st4 = [A.get(F32, [128, 16]) for _ in range(3)]
            swbt = A.get(F32, [128, 2, 256])
            btqs = [Buf("tq0"), Buf("tq1")]
            bpos, bang, btq, bti, bcs, bt1, bt2 = Buf("pos"), Buf("ang"), None, Buf("ti"), [Buf("cs0"), Buf("cs1")], Buf("t1"), Buf("t2")
            bksT, bqsT, bvsw = Buf("ksT"), Buf("qsT"), Buf("vsw")
            bsmx, bpex, bpT = [Buf("smx0"), Buf("smx1")], [Buf("pex0"), Buf("pex1")], [Buf("pT0"), Buf("pT1")]
            bst4 = [Buf("st40"), Buf("st41"), Buf("st42")]
            bswb = Buf("swb")
            sp.dma(swbt, swb[:, :, 0:256].rearrange("v p c -> p v c"), "ld_swb", writes=[bswb])
            nsink = A.get(F32, [128, 8])
            bnsink = Buf("nsink")
            TS(nsink, vec[:, V_SINK:V_SINK + 8], -1.0, None, ALU.mult, None, [bvec], [bnsink])
            xo_v = xo.rearrange("t (a p) f -> t p a f", p=128)
            invf = vec[:, V_INVF:V_INVF + 1]
            sgn = vec[:, V_SGN:V_SGN + 1]

            def p1b_R(i):
                s_ = i % 2
                CP(ang, pos_i, [bpos], [bang])
                if i + 1 < 8:
                    sp.dma(pos_i, posb[i + 1], "ld_pos", writes=[bpos])
                TS(ang, ang, invf, None, ALU.mult, None, [bang, bvec], [bang])
                for (dst, shift, scl, tq, btq) in ((sinT[s_], 0.0, sgn, tqs[0], btqs[0]), (cosT[s_], 0.25, None, tqs[1], btqs[1])):
                    TS(ti, ang, 1.0 / TWO_PI, shift, ALU.mult, ALU.add, [bang], [bti])
                    CP(tq, ti, [bti], [btq])
                    if shift != 0.0:
                        TS(tq, tq, -0.25, None, ALU.add, None, [btq], [btq])
                    STT(tq, tq, -TWO_PI, ang, ALU.mult, ALU.add, [btq, bang], [btq])
                    TS(tq, tq, 3.14159, -3.14159, ALU.min, ALU.max, [btq], [btq])
                    if scl is None:
                        ACT(dst, tq, AF.Sin, [btq], [bcs[s_]], partial=True)
                    else:
                        ACT(dst, tq, AF.Sin, [btq, bvec], [bcs[s_]], scale=scl, partial=False)

            units = []

            def sw_U1(n):
                i, m, j = units[n]
                u, u3 = n % 2, n % 3
                q_ = st4[u3]
                variant = 1 if (i == 0 and m == 0) else 0
                for h in range(2):
                    sbk = 6 if h == 0 else 5
                    MM(PS[sbk][:, 0:256], qsT[h * 64:(h + 1) * 64, j, m * 128:(m + 1) * 128],
                       ksT[h * 64:(h + 1) * 64, m * 128:m * 128 + 256], True, True, [bqsT, bksT], [bPS[sbk]], sig=True)
                for h in range(2):
                    sbk = 6 if h == 0 else 5
                    STT(smx[u][:, h * 256:(h + 1) * 256], PS[sbk][:, 0:256], 0.125, swbt[:, variant, :], ALU.mult, ALU.add, [bPS[sbk], bswb], [bsmx[u]], partial=(h > 0))
                dve.op(lambda e, o_=q_[:, 0:2], i_=smx[u].rearrange("p (h c) -> p h c", h=2): e.tensor_reduce(out=o_, in_=i_, axis=AX.X, op=ALU.max, negate=True),
                       [bsmx[u]], [bst4[u3]])
                TT(q_[:, 2:4], q_[:, 0:2], nsink[:, 2 * j:2 * j + 2], ALU.min, [bst4[u3], bnsink], [bst4[u3]])

            def sw_U2(n):
                i, m, j = units[n]
                u, u3 = n % 2, n % 3
                q_ = st4[u3]
                for h in range(2):
                    ACT(pex[u][:, h * 256:(h + 1) * 256], smx[u][:, h * 256:(h + 1) * 256], AF.Exp, [bsmx[u], bst4[u3]], [bpex[u], bst4[u3]],
                        bias=q_[:, 2 + h:3 + h], accum=q_[:, 6 + h:7 + h], partial=(h > 0))
                for h in range(2):
                    ACT(q_[:, 8 + h:9 + h], q_[:, 2 + h:3 + h], AF.Exp, [bst4[u3], bvec], [bst4[u3]], bias=vec[:, V_SINK + 2 * j + h:V_SINK + 2 * j + h + 1])

            def sw_U2d(n):
                u3 = n % 3
                q_ = st4[u3]
                TT(q_[:, 10:12], q_[:, 6:8], q_[:, 8:10], ALU.add, [bst4[u3]], [bst4[u3]])
                dve.op(lambda e, o_=q_[:, 12:14], i_=q_[:, 10:12]: e.reciprocal(out=o_, in_=i_), [bst4[u3]], [bst4[u3]])

            def sw_U2b(n):
                u = n % 2
                for h in range(2):
                    for half in range(2):
                        c = (h * 2 + half) * 128
                        TR(PSB[7][:, c:c + 128], pex[u][:, h * 256 + half * 128:h * 256 + (half + 1) * 128], identb, [bpex[u], bconst], [bPS[7]],
                           sig=(h == 1 and half == 1))
                ACT(pT[u], PSB[7][:, 0:512], AF.Copy, [bPS[7]], [bpT[u]])

            def sw_U3(n):
                i, m, j = units[n]
                u, u3 = n % 2, n % 3
                q_ = st4[u3]
                s_ = i % 2
                bank = 2 + u
                for h in range(2):
                    for half in range(2):
                        c = (h * 2 + half) * 128
                        MM(PS[bank][:, h * 64:(h + 1) * 64], pT[u][:, c:c + 128], vsw[:, m + half, h * 64:(h + 1) * 64], half == 0, half == 1,
                           [bpT[u], bvsw], [bPS[bank]], sig=(h == 1 and half == 1))
                for h in range(2):
                    head = j + 4 * h
                    TS(ost[s_][:, m, head * 64:(head + 1) * 64], PS[bank][:, h * 64:(h + 1) * 64], q_[:, 12 + h:13 + h], None, ALU.mult, None,
                       [bPS[bank], bst4[u3]], [bost[s_]], partial=True)
                if m == 3 and j == 3:
                    sp.dma(O_s[i * 512:(i + 1) * 512, 512:1024].rearrange("(a p) c -> p a c", p=128), ost[s_], "st_v%d" % s_, reads=[bost[s_]], writes=[bO], partial=True)

            def p1b_B(i):
                s_ = i % 2
                H = hT[s_]
                cT_, sT_ = cosT[s_], sinT[s_]
                for hp in range(4):
                    bank = 2 + hp % 2
                    for k in range(8):
                        MM(PS[bank][:, :], win[:, k, C_Q + hp * 128:C_Q + (hp + 1) * 128], H[:, k, 128:640], k == 0, k == 7,
                           [bwin, bhT[s_]], [bPS[bank]], sig=(k == 7))
                    if hp % 2 == 0:
                        ACT(qst[s_][:, hp, :], PS[bank][:, :], AF.Identity, [bPS[bank]], [bqst[s_]], scale=0.125, partial=(hp > 0))
                    else:
                        TS(qst[s_][:, hp, :], PS[bank][:, :], 0.125, None, ALU.mult, None, [bPS[bank]], [bqst[s_]], partial=True)
                sp.dma(QT_s[:, :, i * 512:(i + 1) * 512].rearrange("h p t -> p h t"), qst[s_], "st_k%d" % s_, reads=[bqst[s_]], writes=[bQT], partial=True)
                for (a0, a1) in ((0, 128), (128, 640)):
                    w_ = a1 - a0
                    for k in range(8):
                        MM(PS[4][:, 0:w_], win[:, k, C_KS:C_KS + 128], H[:, k, a0:a1], k == 0, k == 7, [bwin, bhT[s_]], [bPS[4]], sig=(k == 7))
                    for k in range(8):
                        MM(PS[5][:, 0:w_], win[:, k, C_KSS:C_KSS + 128], H[:, k, a0:a1], k == 0, k == 7, [bwin, bhT[s_]], [bPS[5]], sig=(k == 7))
                    TT(t1[:, 0:w_], PS[4][:, 0:w_], cT_[:, a0:a1], ALU.mult, [bPS[4], bcs[s_]], [bt1])
                    TT(t2[:, 0:w_], PS[5][:, 0:w_], sT_[:, a0:a1], ALU.mult, [bPS[5], bcs[s_]], [bt2])
                    TT(ksT[:, a0:a1], t1[:, 0:w_], t2[:, 0:w_], ALU.add, [bt1, bt2], [bksT], partial=(a0 > 0))
                for j in range(4):
                    for k in range(8):
                        MM(PS[4][:, :], win[:, k, C_QS + j * 128:C_QS + (j + 1) * 128], H[:, k, 128:640], k == 0, k == 7, [bwin, bhT[s_]], [bPS[4]], sig=(k == 7))
                    for k in range(8):
                        MM(PS[5][:, :], win[:, k, C_QSS + j * 128:C_QSS + (j + 1) * 128], H[:, k, 128:640], k == 0, k == 7, [bwin, bhT[s_]], [bPS[5]], sig=(k == 7))
                    TT(t1[:, 0:512], PS[4][:, :], cT_[:, 128:640], ALU.mult, [bPS[4], bcs[s_]], [bt1])
                    TT(t2[:, 0:512], PS[5][:, :], sT_[:, 128:640], ALU.mult, [bPS[5], bcs[s_]], [bt2])
                    TT(qsT[:, j, :], t1[:, 0:512], t2[:, 0:512], ALU.add, [bt1, bt2], [bqsT], partial=(j > 0))
                for a in range(5):
                    bank = 2 + a % 2
                    for k in range(8):
                        MM(PS[bank][:, 0:128], H[:, k, a * 128:(a + 1) * 128], win[:, k, C_VS:C_VS + 128], k == 0, k == 7, [bwin, bhT[s_]], [bPS[bank]], sig=(k == 7))
                    CP(vsw[:, a, :], PS[bank][:, 0:128], [bPS[bank]], [bvsw], partial=(a > 0))
                n0 = len(units)
                for m in range(4):
                    for j in range(4):
                        units.append((i, m, j))
                n1 = len(units)
                for it in range(n0, n1 + 3):
                    if n0 <= it - 3 < n1:
                        sw_U3(it - 3)
                    if n0 <= it - 2 < n1:
                        sw_U2b(it - 2)
                        sw_U2d(it - 2)
                    if n0 <= it - 1 < n1:
                        sw_U2(it - 1)
                    if it < n1:
                        sw_U1(it)

            sp.dma(xt[0], xo_v[0], "ld_x0", writes=[bxt[0]])
            sp.dma(xt[1], xo_v[1], "ld_x1", writes=[bxt[1]])
            sp.dma(pos_i, posb[0], "ld_pos", writes=[bpos])
            for it in range(8 + 3):
                if 0 <= it - 3 < 8:
                    p1b_B(it - 3)
                if 0 <= it - 2 < 8:
                    nt_T3(5, (it - 2) % 2)
                if 0 <= it - 1 < 8:
                    i_ = it - 1
                    nt_T2(5, i_ % 2, i_ % 3)
                    if i_ + 2 < 8:
                        sp.dma(xt[i_ % 2], xo_v[i_ + 2], "ld_x%d" % (i_ % 2), writes=[bxt[i_ % 2]])
                    p1b_R(i_)
                if it < 8:
                    nt_T1(5, it % 2, it % 3)
            fw.barrier()

        if stop_after >= 3:
            A.reset(PERSIST_END)
            KTp = [A.get(BF16, [128, S]) for _ in range(2)]
            Vp = [A.get(BF16, [128, 64, 128]) for _ in range(2)]
            QTp = [A.get(BF16, [128, 4096]) for _ in range(2)]
            amk = A.get(BF16, [128, 16, 512])
            e_t = [A.get(F32, [128, 1024]) for _ in range(2)]
            l_t = [A.get(BF16, [128, 1024]) for _ in range(2)]
            lsum = A.get(F32, [128, 512])
            lsb = [A.get(BF16, [128, 2, 512]) for _ in range(3)]
            a_t = [A.get(BF16, [128, 1024]) for _ in range(2)]
            osb = [A.get(BF16, [128, 4, 128]) for _ in range(2)]
            bKTp, bVp, bQTp = [Buf("KTp0"), Buf("KTp1")], [Buf("Vp0"), Buf("Vp1")], [Buf("QTp0"), Buf("QTp1")]
            bamk = Buf("amk")
            be, bl, blsum, ba, bosb = [Buf("e0"), Buf("e1")], [Buf("l0"), Buf("l1")], Buf("lsum"), [Buf("a0"), Buf("a1")], [Buf("osb0"), Buf("osb1")]
            blsb = [[Buf("lsb%d%d" % (x_, y_)) for y_ in range(2)] for x_ in range(3)]
            bPQ = [Buf("pq%d" % x_) for x_ in range(3)]
            for g in range(4):
                pool.dma(amk[:, g * 4:(g + 1) * 4, :], amask[g * 4:(g + 1) * 4].rearrange("j p c -> p j c"), "ld_amk", writes=[bamk], partial=True)

            def load_pair(hp, s_):
                sp.dma(KTp[s_], KT_s[hp], "ld_kt%d" % s_, reads=[bKT], writes=[bKTp[s_]])
                for g in range(4):
                    sp.dma(Vp[s_][:, g * 16:(g + 1) * 16, :], V_s[g * 2048:(g + 1) * 2048, hp * 128:(hp + 1) * 128].rearrange("(kb p) c -> p kb c", p=128),
                           "ld_v%d" % s_, reads=[bV], writes=[bVp[s_]], partial=(g > 0))
                sp.dma(QTp[s_], QT_s[hp], "ld_qt%d" % s_, reads=[bQT], writes=[bQTp[s_]])

            steps = []
            tid = 0
            for hp in range(4):
                for i in range(8):
                    for hh in range(2):
                        nT = (8 * i + 8) // 2
                        for tl in range(nT):
                            kbA = 8 * i + 7 - 2 * tl
                            steps.append(dict(hp=hp, s=hp % 2, i=i, hh=hh, tl=tl, nT=nT, kb=(kbA, kbA - 1),
                                              tid=tid, osl=(hp * 8 + i) % 2, pfirst=(i == 0 and hh == 0 and tl == 0)))
                        tid += 1
            NS = len(steps)

            def stage1(t):
                d = steps[t]
                u, p3 = t % 2, t % 3
                s_ = d["s"]
                pq = PSP[p3]
                pr = slice(d["hh"] * 64, (d["hh"] + 1) * 64)
                q_ap = QTp[s_][pr, d["i"] * 512:(d["i"] + 1) * 512]
                for half in range(2):
                    kb = d["kb"][half]
                    masked = kb >= 8 * d["i"]
                    dst = pq[:, half * 512:(half + 1) * 512]
                    MM(dst, KTp[s_][pr, kb * 128:(kb + 1) * 128], q_ap, True, not masked, [bKTp[s_], bQTp[s_]], [bPQ[p3]], sig=(not masked))
                    if masked:
                        MM(dst, identb, amk[:, (d["i"] % 2) * 8 + (kb - 8 * d["i"]), :], False, True, [bconst, bamk], [bPQ[p3]], sig=True)
                ACT(e_t[u], pq[:, :], AF.Exp, [bPQ[p3]], [be[u]])
                ACT(l_t[u], e_t[u], AF.Ln, [be[u]], [bl[u]], bias=1.0)
                if d["tl"] == 0:
                    CP(lsum, l_t[u][:, 0:512], [bl[u]], [blsum])
                else:
                    TT(lsum, lsum, l_t[u][:, 0:512], ALU.add, [blsum, bl[u]], [blsum])
                CP(lsb[p3][:, 0, :], lsum, [blsum], [blsb[p3][0]])
                if d["tl"] + 1 < d["nT"]:
                    TT(lsum, lsum, l_t[u][:, 512:1024], ALU.add, [blsum, bl[u]], [blsum])
                    CP(lsb[p3][:, 1, :], lsum, [blsum], [blsb[p3][1]])

            def stage2(t):
                d = steps[t]
                u, p3 = t % 2, t % 3
                pq = PSP[p3]
                pm = (t - 1) % 3
                MM(pq[:, 0:512], trin, l_t[u][:, 0:512], False, d["tl"] == 0, [bconst, bl[u]], [bPQ[p3]], sig=False, skip=True)
                if d["tl"] > 0:
                    MM(pq[:, 0:512], onesn, lsb[pm][:, 1, :], False, True, [bconst, blsb[pm][1]], [bPQ[p3]], sig=False, skip=True)
                MM(pq[:, 512:1024], trin, l_t[u][:, 512:1024], False, False, [bconst, bl[u]], [bPQ[p3]], sig=False, skip=True)
                MM(pq[:, 512:1024], onesn, lsb[p3][:, 0, :], False, True, [bconst, blsb[p3][0]], [bPQ[p3]], sig=True, skip=True)
                ACT(a_t[u], pq[:, :], AF.Exp, [bPQ[p3]], [ba[u]])

            def stage3(t):
                d = steps[t]
                u = t % 2
                s_ = d["s"]
                ou = d["tid"] % 2
                pso, bpso = PS[6 + ou], bPS[6 + ou]
                hh, osl = d["hh"], d["osl"]
                last = d["tl"] == d["nT"] - 1
                for half in range(2):
                    kb = d["kb"][half]
                    for qb in range(4):
                        MM(pso[:, qb * 64:(qb + 1) * 64], a_t[u][:, half * 512 + qb * 128:half * 512 + (qb + 1) * 128], Vp[s_][:, kb, hh * 64:(hh + 1) * 64],
                           (d["tl"] == 0 and half == 0 and qb == 0), (last and half == 1), [ba[u], bVp[s_]], [bpso], sig=(half == 1 and qb == 3), skip=True)
                if last:
                    CP(osb[osl][:, :, hh * 64:(hh + 1) * 64], pso[:, 0:256].rearrange("p (a d) -> p a d", d=64), [bpso], [bosb[osl]], partial=(hh > 0))
                    if hh == 1:
                        i, hp = d["i"], d["hp"]
                        sp.dma(O_s[i * 512:(i + 1) * 512, hp * 128:(hp + 1) * 128].rearrange("(a p) c -> p a c", p=128), osb[osl], "st_o%d" % osl,
                               reads=[bosb[osl]], writes=[bO], partial=True)

            load_pair(0, 0)
            pending_load = None
            for it in range(NS + 2):
                if it < NS:
                    d = steps[it]
                    if d["pfirst"] and d["hp"] + 1 < 4:
                        pending_load = (it + 2, d["hp"] + 1)
                    stage1(it)
                if 0 <= it - 1 < NS:
                    stage2(it - 1)
                if 0 <= it - 2 < NS:
                    stage3(it - 2)
                if pending_load is not None and it >= pending_load[0]:
                    load_pair(pending_load[1], pending_load[1] % 2)
                    pending_load = None
            fw.barrier()

        if stop_after >= 4:
            A.reset(PERSIST_END)
            wout = A.get(BF16, [128, 8, D])
            wr = A.get(F32, [128, 8, 20])
            ob = [A.get(BF16, [128, D]) for _ in range(2)]
            on = [A.get(BF16, [128, D]) for _ in range(2)]
            mixT = [A.get(BF16, [128, 8, 128]) for _ in range(2)]
            xb = [A.get(F32, [128, D]) for _ in range(2)]
            x1 = [A.get(F32, [128, D]) for _ in range(3)]
            xs2 = [A.get(F32, [128, D]) for _ in range(2)]
            s4x = [A.get(F32, [128, 8]) for _ in range(3)]
            h2fs = [A.get(F32, [128, 8, 128]) for _ in range(2)]
            h2b = [A.get(BF16, [128, 8, 128]) for _ in range(2)]
            junk4 = A.get(F32, [128, D])
            s4 = [A.get(F32, [128, 8]) for _ in range(3)]
            rl = [A.get(F32, [128, 32]) for _ in range(2)]
            rt = [A.get(F32, [128, 64]) for _ in range(3)]
            bwout, bwr = Buf("wout"), Buf("wr")
            bob, bon, bmixT = [Buf("ob0"), Buf("ob1")], [Buf("on0"), Buf("on1")], [Buf("mixT0"), Buf("mixT1")]
            bxb, bx1, bxs2, bh2f, bh2b = [Buf("xb0"), Buf("xb1")], [Buf("x10"), Buf("x11"), Buf("x12")], [Buf("xs20"), Buf("xs21")], None, [Buf("h2b0"), Buf("h2b1")]
            bs4x = [Buf("s4x0"), Buf("s4x1"), Buf("s4x2")]
            bh2fs = [Buf("h2f0"), Buf("h2f1")]
            bj4, bs4, brl, brt = Buf("junk4"), [Buf("s40"), Buf("s41"), Buf("s42")], [Buf("rl0"), Buf("rl1")], [Buf("rt0"), Buf("rt1"), Buf("rt2")]
            for k in range(8):
                pool.dma(wout[:, k, :], w_out[k * 128:(k + 1) * 128, :], "ld_wout", writes=[bwout], partial=True)
            sp.dma(wr, w_r.rearrange("(k p) n -> p k n", p=128), "ld_wr", writes=[bwr])

            def p4_load(blk, s_):
                sp.dma(ob[s_], O_s[blk * 128:(blk + 1) * 128, :], "ld_ob%d" % s_, reads=[bO], writes=[bob[s_]])

            def p4_load_x(blk):
                i, m = blk // 4, blk % 4
                s_ = blk % 2
                sp.dma(xb[s_], xo[i, 128 + m * 128:256 + m * 128, :], "ld_xb%d" % s_, writes=[bxb[s_]])

            def st_A(blk):
                s_ = blk % 2
                q_ = s4[blk % 3]
                for h in range(2):
                    ACT(on[s_][:, h * 512:(h + 1) * 512], ob[s_][:, h * 512:(h + 1) * 512], AF.Square, [bob[s_]], [bon[s_], bs4[blk % 3]], accum=q_[:, h:h + 1], partial=True)
                rstd_from_ssq(q_[:, 2:4], q_[:, 0:2], 512.0, [bs4[blk % 3]], [bs4[blk % 3]])

            def st_B(blk):
                s_ = blk % 2
                q_ = s4[blk % 3]
                for h in range(2):
                    TS(on[s_][:, h * 512:(h + 1) * 512], ob[s_][:, h * 512:(h + 1) * 512], q_[:, 2 + h:3 + h], None, ALU.mult, None, [bob[s_], bs4[blk % 3]], [bon[s_]], partial=(h > 0))
                if blk + 2 < 32:
                    p4_load(blk + 2, s_)

            def st_C(blk):
                s_ = blk % 2
                for k in range(8):
                    TR(PSB[0][:, k * 128:(k + 1) * 128], on[s_][:, k * 128:(k + 1) * 128], identb, [bon[s_], bconst], [bPS[0]], sig=(k == 7))
                TT(mixT[s_], PSB[0][:, :].rearrange("p (k t) -> p k t", t=128), G_o, ALU.mult, [bPS[0], bgates], [bmixT[s_]])

            def st_D(blk):
                s_ = blk % 2
                X1 = x1[blk % 3]
                bX = bx1[blk % 3]
                for nt in range(2):
                    for k in range(8):
                        MM(PS[1 + nt][:, :], mixT[s_][:, k, :], wout[:, k, nt * 512:(nt + 1) * 512], k == 0, k == 7, [bmixT[s_], bwout], [bPS[1 + nt]], sig=(k == 7))
                    TT(X1[:, nt * 512:(nt + 1) * 512], PS[1 + nt][:, :], gate1_t[:, nt * 512:(nt + 1) * 512], ALU.mult, [bPS[1 + nt], bgates], [bX], partial=(nt > 0))
                TT(X1, X1, xb[s_], ALU.add, [bX, bxb[s_]], [bX])
                sp.dma(X1_s[blk * 128:(blk + 1) * 128, :], X1, "st_x1%d" % (blk % 3), reads=[bX], writes=[bX1], partial=True)

            def st_E(blk):
                q_ = s4x[blk % 3]
                ACT(junk4, x1[blk % 3], AF.Square, [bx1[blk % 3]], [bj4, bs4x[blk % 3]], accum=q_[:, 0:1], partial=False)
                rstd_from_ssq(q_[:, 1:2], q_[:, 0:1], float(D), [bs4x[blk % 3]], [bs4x[blk % 3]])

            def st_F(blk):
                s_ = blk % 2
                q_ = s4x[blk % 3]
                TS(xs2[s_], x1[blk % 3], q_[:, 1:2], None, ALU.mult, None, [bx1[blk % 3], bs4x[blk % 3]], [bxs2[s_]])

            def st_G(blk):
                s_ = blk % 2
                h2f = h2fs[s_]
                bh2f = bh2fs[s_]
                for half in range(2):
                    bank = 3 + half
                    for kk in range(4):
                        k = half * 4 + kk
                        TR(PS[bank][:, kk * 128:(kk + 1) * 128], xs2[s_][:, k * 128:(k + 1) * 128], identf, [bxs2[s_], bconst], [bPS[bank]], sig=(kk == 3))
                    for kk in range(4):
                        k = half * 4 + kk
                        if half == 0:
                            ACT(h2f[:, k, :], PS[bank][:, kk * 128:(kk + 1) * 128], AF.Identity, [bPS[bank], bmod], [bh2f], bias=sh2[:, k:k + 1], scale=gm2[:, k:k + 1], partial=(k > 0))
                        else:
                            TS(h2f[:, k, :], PS[bank][:, kk * 128:(kk + 1) * 128], gm2[:, k:k + 1], sh2[:, k:k + 1], ALU.mult, ALU.add, [bPS[bank], bmod], [bh2f], partial=True)
                CP(h2b[s_], h2f, [bh2f], [bh2b[s_]])
                sp.dma(H2T_s[:, :, blk * 128:(blk + 1) * 128].rearrange("k p t -> p k t"), h2b[s_], "st_h2%d" % s_, reads=[bh2b[s_]], writes=[bH2T], partial=True)

            def st_H(blk):
                s_ = blk % 2
                h2f = h2fs[s_]
                bh2f = bh2fs[s_]
                for k in range(8):
                    MM(PS[5][:, 0:20], h2f[:, k, :], wr[:, k, :], k == 0, k == 7, [bh2f, bwr], [bPS[5]], sig=(k == 7))
                CP(rl[s_][:, 0:20], PS[5][:, 0:20], [bPS[5]], [brl[s_]])

            def st_I(blk):
                s_ = blk % 2
                R, bR = rt[blk % 3], brt[blk % 3]
                L_, bL = rl[s_], brl[s_]
                dve.op(lambda e, o_=R[:, 0:1], i_=L_[:, 0:4]: e.tensor_reduce(out=o_, in_=i_, axis=AX.X, op=ALU.max), [bL], [bR])
                TS(R[:, 1:2], R[:, 0:1], -1.0, None, ALU.mult, None, [bR], [bR])
                TS(R[:, 8:12], L_[:, 0:4], R[:, 0:1], None, ALU.is_ge, None, [bL, bR], [bR])
                TS(R[:, 12:16], R[:, 8:12], BIG, -BIG, ALU.mult, ALU.add, [bR], [bR])
                for g in range(4):
                    TS(R[:, 16 + 4 * g:20 + 4 * g], L_[:, 4 + 4 * g:8 + 4 * g], R[:, 8 + g:9 + g], R[:, 12 + g:13 + g], ALU.mult, ALU.add, [bL, bR], [bR])
                dve.op(lambda e, o_=R[:, 32:40], i_=R[:, 16:32]: e.max(out=o_, in_=i_), [bR], [bR])
                TS(R[:, 40:56], R[:, 16:32], R[:, 33:34], None, ALU.is_ge, None, [bR], [bR])
                TS(R[:, 3:4], R[:, 32:33], -1.0, None, ALU.mult, None, [bR], [bR])
                TT(R[:, 56:57], R[:, 33:34], R[:, 32:33], ALU.subtract, [bR], [bR])
                CP(R[:, 60:64], L_[:, 0:4], [bL, bR], [bR])

            def st_J(blk):
                R, bR = rt[blk % 3], brt[blk % 3]
                ACT(R[:, 4:8], R[:, 60:64], AF.Exp, [bR], [bR], bias=R[:, 1:2], accum=R[:, 2:3])
                ACT(R[:, 16:32], R[:, 16:32], AF.Exp, [bR], [bR], bias=R[:, 3:4])
                ACT(R[:, 57:58], R[:, 56:57], AF.Exp, [bR], [bR])

            def st_K(blk):
                R, bR = rt[blk % 3], brt[blk % 3]
                TS(R[:, 58:59], R[:, 57:58], 1.0, R[:, 2:3], ALU.add, ALU.mult, [bR], [bR])
                dve.op(lambda e, o_=R[:, 59:60], i_=R[:, 58:59]: e.reciprocal(out=o_, in_=i_), [bR], [bR])
                STT(GW[:, blk, :], R[:, 16:32], R[:, 59:60], R[:, 40:56], ALU.mult, ALU.mult, [bR], [bGW], partial=True)

            stages = [st_A, st_B, st_C, st_D, st_E, st_F, st_G, st_H, st_I, st_J, st_K]
            p4_load(0, 0)
            p4_load(1, 1)
            p4_load_x(0)
            p4_load_x(1)
            for it in range(32 + len(stages) - 1):
                for j in range(len(stages) - 1, -1, -1):
                    blk = it - j
                    if 0 <= blk < 32:
                        stages[j](blk)
                        if j == 3 and blk + 2 < 32:
                            p4_load_x(blk + 2)
            if "GW_s" in dbg:
                sp.dma(GW_s, GW.rearrange("p a b -> p (a b)"), "st_dbg", reads=[bGW], writes=[bDBG], partial=True)
            fw.barrier()

        if stop_after >= 5:
            A.reset(PERSIST_END)
            acc = A.get(F32, [128, 16, D])
            h2T = A.get(BF16, [128, 8, 2048])
            wg = [A.get(BF16, [128, 8, 512]) for _ in range(2)]
            wu = [A.get(BF16, [128, 8, 512]) for _ in range(2)]
            wd = [A.get(BF16, [128, 4, D]) for _ in range(2)]
            wds = A.get(F32, [128, 4, D])
            hid = [A.get(BF16, [128, 4, 512]) for _ in range(2)]
            sg = [A.get(F32, [128, 512]) for _ in range(2)]
            fo = [A.get(F32, [128, D]) for _ in range(2)]
            s5 = [A.get(F32, [128, 4]) for _ in range(2)]
            junk5 = sg[0]
            bacc = [Buf("acc%d" % b_) for b_ in range(16)]
            bwg, bwu, bwd, bwds = [Buf("wg0"), Buf("wg1")], [Buf("wu0"), Buf("wu1")], [Buf("wd0"), Buf("wd1")], Buf("wds")
            bhid, bsg, bfo, bs5 = [Buf("hid0"), Buf("hid1")], [Buf("sg0"), Buf("sg1")], [Buf("fo0"), Buf("fo1")], [Buf("s50"), Buf("s51")]

            def load_w(e_, s_):
                pool.dma(wg[s_], w_gate[e_].rearrange("(k p) n -> p k n", p=128), "ld_wg%d" % s_, writes=[bwg[s_]])
                pool.dma(wu[s_], w_up[e_].rearrange("(k p) n -> p k n", p=128), "ld_wu%d" % s_, writes=[bwu[s_]])
                sp.dma(wds, w_down[e_].rearrange("(m p) f -> p m f", p=128), "ld_wds", writes=[bwds])
                for m in range(4):
                    TT(wd[s_][:, m, :], wds[:, m, :], gate2_t, ALU.mult, [bwds, bgates], [bwd[s_]], partial=(m > 0), eng=pool)

            gcnt = 0
            ycnt = 0
            wcnt = 0
            bh2Tt = [Buf("h2T%d" % x_) for x_ in range(4)]

            def load_acc(sb, g):
                sp.dma(acc[:, g * 4:(g + 1) * 4, :], X1_s[sb * 2048 + g * 512:sb * 2048 + (g + 1) * 512, :].rearrange("(a p) f -> p a f", p=128),
                       "ld_acc%d" % g, reads=[bX1], writes=[bacc[g * 4 + a] for a in range(4)])

            def load_h2T(sb, tt_):
                sp.dma(h2T[:, :, tt_ * 512:(tt_ + 1) * 512], H2T_s[:, :, sb * 2048 + tt_ * 512:sb * 2048 + (tt_ + 1) * 512].rearrange("k p t -> p k t"),
                       "ld_h2T%d" % tt_, reads=[bH2T], writes=[bh2Tt[tt_]])

            def final_norm(sb, blk):
                f_ = blk % 2
                q_ = s5[f_]
                ACT(fo[f_], acc[:, blk, :], AF.Square, [bacc[blk]], [bfo[f_], bs5[f_]], accum=q_[:, 0:1])
                rstd_from_ssq(q_[:, 1:2], q_[:, 0:1], float(D), [bs5[f_]], [bs5[f_]])
                STT(fo[f_], acc[:, blk, :], q_[:, 1:2], gfin_t, ALU.mult, ALU.mult, [bacc[blk], bs5[f_], bgates], [bfo[f_]])
                sp.dma(out[sb * 2048 + blk * 128:sb * 2048 + (blk + 1) * 128, :], fo[f_], "st_out%d" % f_, reads=[bfo[f_]], writes=[bOUT], partial=True)

            jobs = [(sb, e_, tt_) for sb in range(2) for e_ in range(NEXP) for tt_ in range(4)]
            NJ = len(jobs)

            def wslot(sb, e_):
                return (sb * NEXP + e_) % 2

            def job_GU(g):
                sb, e_, tt_ = jobs[g]
                ws = wslot(sb, e_)
                hs = g % 2
                tok = slice(tt_ * 512, (tt_ + 1) * 512)
                for m in range(4):
                    gu = (g * 4 + m) % 2
                    for k in range(8):
                        MM(PS[gu][:, :], wg[ws][:, k, m * 128:(m + 1) * 128], h2T[:, k, tok], k == 0, k == 7, [bwg[ws], bh2Tt[tt_]], [bPS[gu]], sig=(k == 7))
                    for k in range(8):
                        MM(PS[2 + gu][:, :], wu[ws][:, k, m * 128:(m + 1) * 128], h2T[:, k, tok], k == 0, k == 7, [bwu[ws], bh2Tt[tt_]], [bPS[2 + gu]], sig=(k == 7))
                    ACT(sg[gu], PS[gu][:, :], AF.Silu, [bPS[gu]], [bsg[gu]])
                    TT(hid[hs][:, m, :], sg[gu], PS[2 + gu][:, :], ALU.mult, [bsg[gu], bPS[2 + gu]], [bhid[hs]], partial=(m > 0))
                if e_ == NEXP - 1 and sb == 0:
                    load_h2T(1, tt_)

            def job_D(g):
                sb, e_, tt_ = jobs[g]
                ws = wslot(sb, e_)
                hs = g % 2
                for a in range(4):
                    blk = tt_ * 4 + a
                    gblk = sb * 16 + blk
                    for nt in range(2):
                        yb = 4 + (g * 8 + a * 2 + nt) % 4
                        for m in range(4):
                            MM(PS[yb][:, :], hid[hs][:, m, a * 128:(a + 1) * 128], wd[ws][:, m, nt * 512:(nt + 1) * 512], m == 0, m == 3,
                               [bhid[hs], bwd[ws]], [bPS[yb]], sig=(m == 3))
                        STT(acc[:, blk, nt * 512:(nt + 1) * 512], PS[yb][:, :], GW[:, gblk, e_:e_ + 1], acc[:, blk, nt * 512:(nt + 1) * 512], ALU.mult, ALU.add,
                            [bPS[yb], bGW, bacc[blk]], [bacc[blk]], partial=True)
                if e_ == NEXP - 1:
                    for a in range(4):
                        final_norm(sb, tt_ * 4 + a)
                    if sb == 0:
                        load_acc(1, tt_)
                if tt_ == 3:
                    nxt = sb * NEXP + e_ + 2
                    if nxt < 2 * NEXP:
                        load_w(nxt % NEXP, ws)

            load_h2T(0, 0)
            load_w(0, 0)
            for g in range(1, 4):
                load_h2T(0, g)
            load_w(1, 1)
            for g in range(4):
                load_acc(0, g)
            job_GU(0)
            for g in range(NJ):
                if g + 1 < NJ:
                    job_GU(g + 1)
                job_D(g)

        for q in (sp,):
            q.wait_all([bOUT, bDBG, bKT, bV, bQT, bO, bX1, bH2T])
        fw.finish()
        stats = {e.name: e.n_instr for e in fw.engs}
    return nc, stats


def _consts():
    j = np.arange(128)
    ident = np.eye(128, dtype=np.float32)
    trin = np.where(j[:, None] >= j[None, :], -1.0, 0.0).astype(np.float32)
    onesn = -np.ones((128, 128), np.float32)
    ones = np.ones((128, 128), np.float32)
    return np.stack([ident, trin, onesn, ones])


def _amask(r):
    out = np.zeros((2, 8, 128, 512), np.float32)
    sl = np.arange(128)[:, None]
    tl = np.arange(512)[None, :]
    for par in range(2):
        v = par if r == 0 else 1 - par
        for jj in range(8):
            valid = (128 * jj + sl) < (512 * v + tl)
            out[par, jj] = np.where(valid, 0.0, NEG)
    return out.reshape(16, 128, 512)


def _swb(r):
    qi = np.arange(128)[:, None]
    kk = np.arange(256)[None, :]
    diff = 128 + qi - kk
    ok = (diff >= 0) & (diff < 128)
    norm = np.where(ok, 0.0, NEG).astype(np.float32)
    first = np.where(ok & (kk >= 128), 0.0, NEG).astype(np.float32) if r == 0 else norm
    return np.stack([np.concatenate([norm, norm], 1), np.concatenate([first, first], 1)])


def _prep_inputs(x, c, positions, w_ada, b_ada, norm_mix_g, w_in, sinks, out_norm_sb_g, out_norm_sw_g,
                 w_out, norm_ffn_g, w_router_group, w_router_expert, w_gate, w_up, w_down, norm_final_g):
    f32 = np.float32
    x = np.asarray(x, f32)
    positions = np.asarray(positions, np.int32)
    w_in0 = np.asarray(w_in[0], f32)

    def chunkT(v):
        return np.ascontiguousarray(np.asarray(v, f32).reshape(-1, 128).T)

    qs = w_in0[:, 1536:2048].reshape(D, 8, 64)
    qs_perm = np.concatenate([np.concatenate([qs[:, j], qs[:, 4 + j]], axis=1) for j in range(4)], axis=1)
    qs_sw = np.concatenate([qs[:, :, 32:], qs[:, :, :32]], axis=2)
    qs_sw_perm = np.concatenate([np.concatenate([qs_sw[:, j], qs_sw[:, 4 + j]], axis=1) for j in range(4)], axis=1)
    ks = w_in0[:, 2048:2176].reshape(D, 2, 64)
    ks_sw = np.concatenate([ks[:, :, 32:], ks[:, :, :32]], axis=2).reshape(D, 128)
    w_inr = np.ascontiguousarray(np.concatenate([
        w_in0[:, 0:512], w_in0[:, 512:1024], w_in0[:, 1024:1536], qs_perm, qs_sw_perm,
        w_in0[:, 2048:2176], ks_sw, w_in0[:, 2176:2304]], axis=1))
    assert w_inr.shape[1] == C_END
    sk = np.asarray(sinks[0], f32)
    sink_cols = np.stack([sk[[j, 4 + j]] for j in range(4)]).reshape(8)
    inv_freq = (10000.0 ** (-np.arange(0, 64, 2, dtype=np.float32) / 64)).astype(f32)
    p = np.arange(128)
    invf = inv_freq[p % 32]
    sgn = np.where((p % 64) < 32, -1.0, 1.0).astype(f32)
    g_o = np.concatenate([np.asarray(out_norm_sb_g[0], f32), np.asarray(out_norm_sw_g[0], f32)])
    cm = _consts()
    w_r = np.ascontiguousarray(np.concatenate([np.asarray(w_router_group[0], f32), np.asarray(w_router_expert[0], f32)], axis=1))
    shared = dict(w_ada=np.ascontiguousarray(np.asarray(w_ada[0], f32)), w_inr=w_inr,
                  w_out=np.ascontiguousarray(np.asarray(w_out[0], f32)), w_r=w_r,
                  w_gate=np.ascontiguousarray(np.asarray(w_gate[0], f32)), w_up=np.ascontiguousarray(np.asarray(w_up[0], f32)),
                  w_down=np.ascontiguousarray(np.asarray(w_down[0], f32)), cmat=cm)
    in_maps = []
    for core in range(8):
        b, r = core // 2, core % 2
        xo = np.zeros((8, 640, D), f32)
        po = np.zeros((8, 640), np.int32)
        for i, qt in enumerate(T_R[r]):
            lo = 512 * qt - 128
            if lo < 0:
                xo[i, 128:] = x[b, 0:512]
                po[i, 128:] = positions[b, 0:512]
            else:
                xo[i] = x[b, lo:lo + 640]
                po[i] = positions[b, lo:lo + 640]
        vecs = np.zeros((128, V_N), f32)
        vecs[:, V_BADA:V_BADA + 48] = np.asarray(b_ada[0], f32).reshape(48, 128).T
        vecs[:, V_GMIX:V_GMIX + 8] = chunkT(norm_mix_g[0])
        vecs[:, V_GFFN:V_GFFN + 8] = chunkT(norm_ffn_g[0])
        vecs[:, V_GO:V_GO + 8] = chunkT(g_o)
        vecs[:, V_GFIN:V_GFIN + 8] = chunkT(norm_final_g)
        vecs[:, V_SINK:V_SINK + 8] = sink_cols[None, :]
        vecs[:, V_INVF] = invf
        vecs[:, V_SGN] = sgn
        m = dict(shared)
        m.update(xs=np.ascontiguousarray(x[b]), xo=xo,
                 posb=np.ascontiguousarray(np.broadcast_to(po[:, None, :], (8, 128, 640))),
                 cT=chunkT(np.asarray(c, f32)[b]), vecs=vecs, amask=_amask(r), swb=_swb(r))
        in_maps.append(m)
    return in_maps


_CACHE = {}


def kernel(**inputs):
    in_maps = _prep_inputs(**inputs)
    if "nc" not in _CACHE:
        _CACHE["nc"] = build_program()[0]
    nc = _CACHE["nc"]
    res = run_bass_kernel_spmd(nc, in_maps, core_ids=list(range(8)))
    out = np.zeros((4, S, D), np.float32)
    for core in range(8):
        b, r = core // 2, core % 2
        o = np.asarray(res.results[core]["out"], np.float32).reshape(8, 512, D)
        for i, qt in enumerate(T_R[r]):
            out[b, 512 * qt:512 * (qt + 1)] = o[i]
    return out
```

```python
import os
import contextlib
import numpy as np
import concourse.bass as bass
import concourse.mybir as mybir
from concourse.bass_utils import run_bass_kernel_spmd

F32 = mybir.dt.float32
BF16 = mybir.dt.bfloat16
I32 = mybir.dt.int32
AF = mybir.ActivationFunctionType
ALU = mybir.AluOpType
AX = mybir.AxisListType

S = 8192
D = 1024
NEXP = 16
NEG = -30000.0
BIG = 1.0e30
EPS = 1e-6
TWO_PI = 6.283185307179586
T_R = ([0, 3, 4, 7, 8, 11, 12, 15], [1, 2, 5, 6, 9, 10, 13, 14])
C_Q, C_K, C_V, C_QS, C_QSS, C_KS, C_KSS, C_VS, C_END = 0, 512, 1024, 1536, 2048, 2560, 2688, 2816, 2944
V_BADA, V_GMIX, V_GFFN, V_GO, V_GFIN, V_SINK, V_INVF, V_SGN, V_N = 0, 48, 56, 64, 72, 80, 88, 89, 90


class Buf:
    __slots__ = ("name", "writers", "readers", "pend")

    def __init__(self, name):
        self.name = name
        self.writers = {}
        self.readers = {}
        self.pend = {}


class Eng:
    def __init__(self, fw, name, is_pe=False, has_sem=True):
        self.fw = fw
        self.name = name
        self.is_pe = is_pe
        self.key = "e_" + name
        self.sem = fw.new_sem(self.key) if has_sem else None
        self.cnt = 0
        self.seen = {}
        self.prog = []
        self.n_instr = 0

    def _wait(self, semkey, val):
        if val <= 0 or self.seen.get(semkey, 0) >= val:
            return
        self.seen[semkey] = val
        sem = self.fw.sems[semkey]
        self.prog.append(lambda e, sem=sem, val=val: e.wait_ge(sem, val))

    def _deps(self, reads, writes, partial):
        for b in reads:
            for k, v in b.writers.items():
                if not (self.is_pe and k == self.key):
                    self._wait(k, v)
        for b in writes:
            for k, v in b.readers.items():
                if not (self.is_pe and k == self.key):
                    self._wait(k, v)
            if not partial:
                for k, v in b.writers.items():
                    if not (self.is_pe and k == self.key):
                        self._wait(k, v)
            else:
                for k, v in b.pend.items():
                    if not (self.is_pe and k == self.key):
                        self._wait(k, v)

    def _mark(self, key, val, reads, writes, partial):
        for b in writes:
            if not partial:
                pend = dict(b.readers)
                for k, v in b.writers.items():
                    pend[k] = max(pend.get(k, 0), v)
                b.pend = pend
                b.writers = {}
                b.readers = {}
            b.writers[key] = max(b.writers.get(key, 0), val)
        for b in reads:
            b.readers[key] = max(b.readers.get(key, 0), val)

    def op(self, fn, reads=(), writes=(), sig=True, partial=False):
        self._deps(reads, writes, partial)
        if sig:
            self.cnt += 1
            assert self.cnt < 60000, self.name
            val = self.cnt
            sem = self.sem
            self.prog.append(lambda e, fn=fn, sem=sem: fn(e).then_inc(sem, 1))
        else:
            val = self.cnt + 1
            self.prog.append(lambda e, fn=fn: fn(e))
        self.n_instr += 1
        self._mark(self.key, val, reads, writes, partial)

    def dma(self, out, in_, semkey, reads=(), writes=(), partial=False, **kw):
        self._deps(reads, writes, partial)
        fw = self.fw
        if semkey not in fw.sems:
            fw.new_sem(semkey)
        fw.semcnt[semkey] = fw.semcnt.get(semkey, 0) + 16
        val = fw.semcnt[semkey]
        assert val < 60000, semkey
        sem = fw.sems[semkey]
        self.prog.append(lambda e, out=out, in_=in_, sem=sem, kw=kw:
                         e.dma_start(out=out, in_=in_, **kw).then_inc(sem, 16))
        self.n_instr += 1
        self._mark(semkey, val, reads, writes, partial)

    def wait_all(self, bufs):
        for b in bufs:
            for k, v in list(b.writers.items()) + list(b.readers.items()):
                self._wait(k, v)


class FW:
    def __init__(self, nc, stack):
        self.nc = nc
        self.stack = stack
        self.sems = {}
        self.semcnt = {}
        self.pe = Eng(self, "pe", is_pe=True)
        self.act = Eng(self, "act")
        self.dve = Eng(self, "dve")
        self.pool = Eng(self, "pool")
        self.sp = Eng(self, "sp", has_sem=False)
        self.engs = [self.pe, self.act, self.dve, self.pool, self.sp]

    def new_sem(self, key):
        s = self.stack.enter_context(self.nc.semaphore(key))
        self.sems[key] = s
        return s

    def barrier(self):
        for e in self.engs:
            for q in (self.pe, self.act, self.dve, self.pool):
                if q is not e:
                    e._wait(q.key, q.cnt)
                elif not e.is_pe:
                    e._wait(q.key, q.cnt)
            for k, v in self.semcnt.items():
                e._wait(k, v)

    def finish(self):
        nc = self.nc
        with nc.Block() as block:
            @block.tensor
            def _(e):
                for t in self.pe.prog:
                    t(e)

            @block.scalar
            def _(e):
                for t in self.act.prog:
                    t(e)

            @block.vector
            def _(e):
                for t in self.dve.prog:
                    t(e)

            @block.gpsimd
            def _(e):
                for t in self.pool.prog:
                    t(e)

            @block.sync
            def _(e):
                for t in self.sp.prog:
                    t(e)


class Arena:
    def __init__(self, nc, nbytes):
        self.nbytes = nbytes
        self.f = nc.alloc_sbuf_tensor("arena", [128, nbytes // 4], F32)
        self.b = self.f.bitcast(BF16)
        self.i = self.f.bitcast(I32)
        self.top = 0

    def reset(self, off):
        self.top = off

    def get(self, dt, shape):
        es = 2 if dt == BF16 else 4
        n = 1
        for s_ in shape[1:]:
            n *= s_
        nb = (n * es + 31) // 32 * 32
        off = self.top
        self.top += nb
        assert self.top <= self.nbytes, ("arena overflow", self.top, self.nbytes)
        h = self.b if dt == BF16 else (self.i if dt == I32 else self.f)
        ap = h[0:shape[0], off // es: off // es + n]
        if len(shape) == 3:
            ap = ap.rearrange("p (a b) -> p a b", b=shape[2])
        return ap


def build_program(debug_outs=(), stop_after=99):
    nc = bass.Bass("TRN2", target_bir_lowering=False)
    dbg = set(debug_outs)

    def din(name, shape, dt=F32):
        return nc.dram_tensor(name, list(shape), dt, kind="ExternalInput").ap()

    def dscr(name, shape, dt):
        kind = "ExternalOutput" if name in dbg else "Internal"
        return nc.dram_tensor(name, list(shape), dt, kind=kind).ap()

    xs = din("xs", [S, D])
    xo = din("xo", [8, 640, D])
    posb = din("posb", [8, 128, 640], I32)
    cT = din("cT", [128, 8])
    w_ada = din("w_ada", [D, 6 * D])
    vecs = din("vecs", [128, V_N])
    w_inr = din("w_inr", [D, C_END])
    w_out = din("w_out", [D, D])
    w_r = din("w_r", [D, 20])
    w_gate = din("w_gate", [NEXP, D, 512])
    w_up = din("w_up", [NEXP, D, 512])
    w_down = din("w_down", [NEXP, 512, D])
    cmat = din("cmat", [4, 128, 128])
    amask = din("amask", [16, 128, 512])
    swb = din("swb", [2, 128, 512])
    out = nc.dram_tensor("out", [4096, D], F32, kind="ExternalOutput").ap()

    KT_s = dscr("KT_s", [4, 128, S], BF16)
    V_s = dscr("V_s", [S, 512], BF16)
    QT_s = dscr("QT_s", [4, 128, 4096], BF16)
    O_s = dscr("O_s", [4096, D], BF16)
    X1_s = dscr("X1_s", [4096, D], F32)
    H2T_s = dscr("H2T_s", [8, 128, 4096], BF16)
    MOD_s = dscr("MOD_s", [128, 48], F32)
    GW_s = dscr("GW_s", [128, 32 * 16], F32)

    with contextlib.ExitStack() as st:
        fw = FW(nc, st)
        pe, act, dve, pool, sp = fw.pe, fw.act, fw.dve, fw.pool, fw.sp
        A = Arena(nc, 211968)
        PSP = [nc.alloc_psum_tensor("psp%d" % i, [128, 1024], F32) for i in range(4)]
        PSPB = [p.bitcast(BF16) for p in PSP]
        PS = [PSP[i // 2][:, (i % 2) * 512:(i % 2 + 1) * 512] for i in range(8)]
        PSB = [PSPB[i // 2][:, (i % 2) * 1024:(i % 2 + 1) * 1024] for i in range(8)]
        bPS = [Buf("ps%d" % i) for i in range(8)]

        def ACT(out_, in_, func, reads, writes, bias=None, scale=None, accum=None, partial=False):
            kw = {}
            if bias is not None:
                kw["bias"] = bias
            if scale is not None:
                kw["scale"] = scale
            if accum is not None:
                kw["accum_out"] = accum
            act.op(lambda e: e.activation(out=out_, in_=in_, func=func, **kw), reads, writes, partial=partial)

        def TS(out_, in0, s1, s2, op0, op1, reads, writes, partial=False, eng=None):
            q = eng or dve
            if op1 is None:
                q.op(lambda e: e.tensor_scalar(out=out_, in0=in0, scalar1=s1, scalar2=None, op0=op0), reads, writes, partial=partial)
            else:
                q.op(lambda e: e.tensor_scalar(out=out_, in0=in0, scalar1=s1, scalar2=s2, op0=op0, op1=op1), reads, writes, partial=partial)

        def TT(out_, in0, in1, op, reads, writes, partial=False, eng=None):
            q = eng or dve
            q.op(lambda e: e.tensor_tensor(out=out_, in0=in0, in1=in1, op=op), reads, writes, partial=partial)

        def STT(out_, in0, scalar, in1, op0, op1, reads, writes, partial=False):
            dve.op(lambda e: e.scalar_tensor_tensor(out=out_, in0=in0, scalar=scalar, in1=in1, op0=op0, op1=op1), reads, writes, partial=partial)

        def CP(out_, in_, reads, writes, partial=False, eng=None):
            q = eng or dve
            q.op(lambda e: e.tensor_copy(out=out_, in_=in_), reads, writes, partial=partial)

        def MM(out_, lhsT, rhs, start, stop, reads, writes, sig=False, skip=False):
            if skip:
                pe.op(lambda e: e.matmul(out_, lhsT=lhsT, rhs=rhs, start=start, stop=stop, skip_group_check=True), reads, writes, sig=sig, partial=True)
            else:
                pe.op(lambda e: e.matmul(out_, lhsT=lhsT, rhs=rhs, start=start, stop=stop), reads, writes, sig=sig, partial=True)

        def TR(out_, in_, ident, reads, writes, sig=False):
            pe.op(lambda e: e.transpose(out_, in_, ident), reads, writes, sig=sig, partial=True)

        def rstd_from_ssq(dst, ssq, n, reads, writes):
            ACT(dst, ssq, AF.Ln, reads, writes, bias=epsc, scale=1.0 / n)
            ACT(dst, dst, AF.Exp, writes, writes, scale=-0.5)

        identf = A.get(F32, [128, 128])
        onesf = A.get(F32, [128, 128])
        identb = A.get(BF16, [128, 128])
        trin = A.get(BF16, [128, 128])
        onesn = A.get(BF16, [128, 128])
        vec = A.get(F32, [128, V_N])
        modT = A.get(F32, [128, 48])
        gm1 = A.get(F32, [128, 8])
        gm2 = A.get(F32, [128, 8])
        epsc = A.get(F32, [128, 1])
        gate1_t = A.get(F32, [128, D])
        gate2_t = A.get(F32, [128, D])
        gfin_t = A.get(F32, [128, D])
        G_o = A.get(BF16, [128, 8, 128])
        GW = A.get(F32, [128, 32, 16])
        PERSIST_END = A.top
        bconst = Buf("const")
        bvec = Buf("vec")
        bmod = Buf("mod")
        bgates = Buf("gates")
        bGW = Buf("GW")
        bKT, bV, bQT, bO, bX1, bH2T = Buf("KT_s"), Buf("V_s"), Buf("QT_s"), Buf("O_s"), Buf("X1_s"), Buf("H2T_s")
        bOUT = Buf("out")
        bDBG = Buf("dbg")

        sh1 = modT[:, 0:8]
        sh2 = modT[:, 24:32]

        sp.dma(identf, cmat[0], "ld_cf", writes=[bconst])
        sp.dma(onesf, cmat[3], "ld_cf", writes=[bconst], partial=True)
        pool.dma(identb, cmat[0], "ld_cb", writes=[bconst], partial=True)
        pool.dma(trin, cmat[1], "ld_cb", writes=[bconst], partial=True)
        pool.dma(onesn, cmat[2], "ld_cb", writes=[bconst], partial=True)
        sp.dma(vec, vecs, "ld_vec", writes=[bvec])
        dve.op(lambda e: e.memset(epsc, EPS), (), [bconst], partial=True)

        A.reset(PERSIST_END)
        sc_in = A.get(F32, [128, 8])
        sc = A.get(F32, [128, 8])
        wa = [A.get(F32, [128, 8, 512]) for _ in range(2)]
        diag = [A.get(F32, [128, 128]) for _ in range(2)]
        bsc, bwa, bdiag = Buf("sc"), [Buf("wa0"), Buf("wa1")], [Buf("dg0"), Buf("dg1")]
        sp.dma(sc_in, cT, "ld_sc", writes=[bsc])
        ACT(sc, sc_in, AF.Silu, [bsc], [bsc])
        w_ada_v = w_ada.rearrange("(k p) n -> p k n", p=128)
        for nt in range(12):
            s_ = nt % 2
            sp.dma(wa[s_], w_ada_v[:, :, nt * 512:(nt + 1) * 512], "ld_wa%d" % s_, writes=[bwa[s_]])
            for m in range(4):
                j = 4 * nt + m
                for k in range(8):
                    MM(PS[0][:, j:j + 1], wa[s_][:, k, m * 128:(m + 1) * 128], sc[:, k:k + 1], k == 0, k == 7,
                       [bwa[s_], bsc], [bPS[0]], sig=(k == 7))
        TT(modT, PS[0][:, 0:48], vec[:, V_BADA:V_BADA + 48], ALU.add, [bPS[0], bvec], [bmod])
        STT(gm1, modT[:, 8:16], 1.0, vec[:, V_GMIX:V_GMIX + 8], ALU.add, ALU.mult, [bmod, bvec], [bmod], partial=True)
        STT(gm2, modT[:, 32:40], 1.0, vec[:, V_GFFN:V_GFFN + 8], ALU.add, ALU.mult, [bmod, bvec], [bmod], partial=True)
        nd = 0
        for (dst, src) in ((gate1_t, modT[:, 16:24]), (gate2_t, modT[:, 40:48]), (gfin_t, vec[:, V_GFIN:V_GFIN + 8])):
            for half in range(2):
                bank = 1 + half
                for kk in range(4):
                    k = half * 4 + kk
                    d_ = nd % 2
                    nd += 1
                    TS(diag[d_], identf, src[:, k:k + 1], None, ALU.mult, None, [bconst, bmod, bvec], [bdiag[d_]])
                    MM(PS[bank][:, kk * 128:(kk + 1) * 128], onesf, diag[d_], True, True, [bconst, bdiag[d_]], [bPS[bank]], sig=True)
                CP(dst[:, half * 512:(half + 1) * 512], PS[bank][:, :], [bPS[bank]], [bgates], partial=True)
        for k in range(8):
            TS(G_o[:, k, :], onesf, vec[:, V_GO + k:V_GO + k + 1], None, ALU.mult, None, [bconst, bvec], [bgates], partial=True)
        if "MOD_s" in dbg:
            sp.dma(MOD_s, modT, "st_dbg", reads=[bmod], writes=[bDBG], partial=True)
        fw.barrier()

        if stop_after >= 1:
            A.reset(PERSIST_END)
            win = A.get(BF16, [128, 8, C_END])
            xt = [A.get(F32, [128, 5, D]) for _ in range(2)]
            xn = [A.get(BF16, [128, 5, D]) for _ in range(2)]
            hT = [A.get(BF16, [128, 8, 640]) for _ in range(2)]
            ssq = [A.get(F32, [128, 8]) for _ in range(3)]
            rstd = [A.get(F32, [128, 8]) for _ in range(3)]
            kst = [A.get(BF16, [128, 4, 512]) for _ in range(2)]
            vst = [A.get(BF16, [128, 4, 512]) for _ in range(2)]
            bwin = Buf("win")
            bxt, bxn, bhT = [Buf("xt0"), Buf("xt1")], [Buf("xn0"), Buf("xn1")], [Buf("hT0"), Buf("hT1")]
            bjunk, bssq = Buf("junk"), [Buf("ssq0"), Buf("ssq1"), Buf("ssq2")]
            bkst, bvst = [Buf("kst0"), Buf("kst1")], [Buf("vst0"), Buf("vst1")]
            for k in range(8):
                for (c0, c1) in ((0, 1024), (1024, 2048), (2048, C_END)):
                    pool.dma(win[:, k, c0:c1], w_inr[k * 128:(k + 1) * 128, c0:c1], "ld_win", writes=[bwin], partial=True)

            def nt_T1(nblk, s_, q3):
                X, XN = xt[s_], xn[s_]
                for a in range(nblk):
                    ACT(XN[:, a, :], X[:, a, :], AF.Square, [bxt[s_]], [bxn[s_], bssq[q3]], accum=ssq[q3][:, a:a + 1], partial=True)
                rstd_from_ssq(rstd[q3][:, 0:nblk], ssq[q3][:, 0:nblk], float(D), [bssq[q3]], [bssq[q3]])

            def nt_T2(nblk, s_, q3):
                X, XN = xt[s_], xn[s_]
                for a in range(nblk):
                    TS(XN[:, a, :], X[:, a, :], rstd[q3][:, a:a + 1], None, ALU.mult, None, [bxt[s_], bssq[q3]], [bxn[s_]], partial=(a > 0))

            def nt_T3(nblk, s_):
                XN = xn[s_]
                W = nblk * 128
                for k in range(8):
                    bank = k % 2
                    for a in range(nblk):
                        TR(PSB[bank][:, a * 128:(a + 1) * 128], XN[:, a, k * 128:(k + 1) * 128], identb, [bxn[s_], bconst], [bPS[bank]], sig=(a == nblk - 1))
                    if bank == 0:
                        ACT(hT[s_][:, k, 0:W], PSB[bank][:, 0:W], AF.Identity, [bPS[bank], bmod], [bhT[s_]], bias=sh1[:, k:k + 1], scale=gm1[:, k:k + 1], partial=(k > 0))
                    else:
                        TS(hT[s_][:, k, 0:W], PSB[bank][:, 0:W], gm1[:, k:k + 1], sh1[:, k:k + 1], ALU.mult, ALU.add, [bPS[bank], bmod], [bhT[s_]], partial=True)

            xs_v = xs.rearrange("(t a p) f -> t p a f", p=128, a=4)

            def p1a_B(i):
                s_ = i % 2
                H = hT[s_]
                for hp in range(4):
                    bank = 2 + hp % 2
                    for k in range(8):
                        MM(PS[bank][:, :], win[:, k, C_K + hp * 128:C_K + (hp + 1) * 128], H[:, k, 0:512], k == 0, k == 7,
                           [bwin, bhT[s_]], [bPS[bank]], sig=(k == 7))
                    if hp % 2 == 0:
                        ACT(kst[s_][:, hp, :], PS[bank][:, :], AF.Copy, [bPS[bank]], [bkst[s_]], partial=(hp > 0))
                    else:
                        CP(kst[s_][:, hp, :], PS[bank][:, :], [bPS[bank]], [bkst[s_]], partial=True)
                pool.dma(KT_s[:, :, i * 512:(i + 1) * 512].rearrange("h p t -> p h t"), kst[s_], "st_k%d" % s_, reads=[bkst[s_]], writes=[bKT], partial=True)
                for a in range(4):
                    bank = 4 + a % 2
                    for k in range(8):
                        MM(PS[bank][:, :], H[:, k, a * 128:(a + 1) * 128], win[:, k, C_V:C_V + 512], k == 0, k == 7,
                           [bwin, bhT[s_]], [bPS[bank]], sig=(k == 7))
                    if a % 2 == 0:
                        ACT(vst[s_][:, a, :], PS[bank][:, :], AF.Copy, [bPS[bank]], [bvst[s_]], partial=(a > 0))
                    else:
                        CP(vst[s_][:, a, :], PS[bank][:, :], [bPS[bank]], [bvst[s_]], partial=True)
                pool.dma(V_s[i * 512:(i + 1) * 512, :].rearrange("(a p) c -> p a c", p=128), vst[s_], "st_v%d" % s_, reads=[bvst[s_]], writes=[bV], partial=True)

            sp.dma(xt[0][:, 0:4, :], xs_v[0], "ld_x0", writes=[bxt[0]])
            sp.dma(xt[1][:, 0:4, :], xs_v[1], "ld_x1", writes=[bxt[1]])
            for it in range(16 + 3):
                if 0 <= it - 3 < 16:
                    p1a_B(it - 3)
                if 0 <= it - 2 < 16:
                    nt_T3(4, (it - 2) % 2)
                if 0 <= it - 1 < 16:
                    i_ = it - 1
                    nt_T2(4, i_ % 2, i_ % 3)
                    if i_ + 2 < 16:
                        sp.dma(xt[i_ % 2][:, 0:4, :], xs_v[i_ + 2], "ld_x%d" % (i_ % 2), writes=[bxt[i_ % 2]])
                if it < 16:
                    nt_T1(4, it % 2, it % 3)

            qst = kst
            bqst = bkst
            ost = vst
            bost = bvst
            pos_i = A.get(I32, [128, 640])
            ang = A.get(F32, [128, 640])
            tqs = [A.get(F32, [128, 640]) for _ in range(2)]
            ti = A.get(I32, [128, 640])
            cosT = [A.get(F32, [128, 640]) for _ in range(2)]
            sinT = [A.get(F32, [128, 640]) for _ in range(2)]
            t1 = A.get(F32, [128, 512])
            t2 = A.get(F32, [128, 512])
            ksT = A.get(BF16, [128, 640])
            qsT = A.get(BF16, [128, 4, 512])
            vsw = A.get(BF16, [128, 5, 128])
            smx = [A.get(F32, [128, 512]) for _ in range(2)]
            pex = [A.get(BF16, [128, 512]) for _ in range(2)]
            pT = [A.get(BF16, [128, 512]) for _ in range(2)]
            st4 = [A.get(F32, [128, 16]) for _ in range(3)]
            swbt = A.get(F32, [128, 2, 256])
            btqs = [Buf("tq0"), Buf("tq1")]
            bpos, bang, btq, bti, bcs, bt1, bt2 = Buf("pos"), Buf("ang"), None, Buf("ti"), [Buf("cs0"), Buf("cs1")], Buf("t1"), Buf("t2")
            bksT, bqsT, bvsw = Buf("ksT"), Buf("qsT"), Buf("vsw")
            bsmx, bpex, bpT = [Buf("smx0"), Buf("smx1")], [Buf("pex0"), Buf("pex1")], [Buf("pT0"), Buf("pT1")]
            bst4 = [Buf("st40"), Buf("st41"), Buf("st42")]
            bswb = Buf("swb")
            sp.dma(swbt, swb[:, :, 0:256].rearrange("v p c -> p v c"), "ld_swb", writes=[bswb])
            nsink = A.get(F32, [128, 8])
            bnsink = Buf("nsink")
            TS(nsink, vec[:, V_SINK:V_SINK + 8], -1.0, None, ALU.mult, None, [bvec], [bnsink])
            xo_v = xo.rearrange("t (a p) f -> t p a f", p=128)
            invf = vec[:, V_INVF:V_INVF + 1]
            sgn = vec[:, V_SGN:V_SGN + 1]

            def p1b_R(i):
                s_ = i % 2
                CP(ang, pos_i, [bpos], [bang])
                if i + 1 < 8:
                    sp.dma(pos_i, posb[i + 1], "ld_pos", writes=[bpos])
                TS(ang, ang, invf, None, ALU.mult, None, [bang, bvec], [bang])
                for (dst, shift, scl, tq, btq) in ((sinT[s_], 0.0, sgn, tqs[0], btqs[0]), (cosT[s_], 0.25, None, tqs[1], btqs[1])):
                    TS(ti, ang, 1.0 / TWO_PI, shift, ALU.mult, ALU.add, [bang], [bti])
                    CP(tq, ti, [bti], [btq])
                    if shift != 0.0:
                        TS(tq, tq, -0.25, None, ALU.add, None, [btq], [btq])
                    STT(tq, tq, -TWO_PI, ang, ALU.mult, ALU.add, [btq, bang], [btq])
                    TS(tq, tq, 3.14159, -3.14159, ALU.min, ALU.max, [btq], [btq])
                    if scl is None:
                        ACT(dst, tq, AF.Sin, [btq], [bcs[s_]], partial=True)
                    else:
                        ACT(dst, tq, AF.Sin, [btq, bvec], [bcs[s_]], scale=scl, partial=False)

            units = []

            def sw_U1(n):
                i, m, j = units[n]
                u, u3 = n % 2, n % 3
                q_ = st4[u3]
                variant = 1 if (i == 0 and m == 0) else 0
                for h in range(2):
                    sbk = 6 if h == 0 else 5
                    MM(PS[sbk][:, 0:256], qsT[h * 64:(h + 1) * 64, j, m * 128:(m + 1) * 128],
                       ksT[h * 64:(h + 1) * 64, m * 128:m * 128 + 256], True, True, [bqsT, bksT], [bPS[sbk]], sig=True)
                for h in range(2):
                    sbk = 6 if h == 0 else 5
                    STT(smx[u][:, h * 256:(h + 1) * 256], PS[sbk][:, 0:256], 0.125, swbt[:, variant, :], ALU.mult, ALU.add, [bPS[sbk], bswb], [bsmx[u]], partial=(h > 0))
                dve.op(lambda e, o_=q_[:, 0:2], i_=smx[u].rearrange("p (h c) -> p h c", h=2): e.tensor_reduce(out=o_, in_=i_, axis=AX.X, op=ALU.max, negate=True),
                       [bsmx[u]], [bst4[u3]])
                TT(q_[:, 2:4], q_[:, 0:2], nsink[:, 2 * j:2 * j + 2], ALU.min, [bst4[u3], bnsink], [bst4[u3]])

            def sw_U2(n):
                i, m, j = units[n]
                u, u3 = n % 2, n % 3
                q_ = st4[u3]
                for h in range(2):
                    ACT(pex[u][:, h * 256:(h + 1) * 256], smx[u][:, h * 256:(h + 1) * 256], AF.Exp, [bsmx[u], bst4[u3]], [bpex[u], bst4[u3]],
                        bias=q_[:, 2 + h:3 + h], accum=q_[:, 6 + h:7 + h], partial=(h > 0))
                for h in range(2):
                    ACT(q_[:, 8 + h:9 + h], q_[:, 2 + h:3 + h], AF.Exp, [bst4[u3], bvec], [bst4[u3]], bias=vec[:, V_SINK + 2 * j + h:V_SINK + 2 * j + h + 1])

            def sw_U2d(n):
                u3 = n % 3
                q_ = st4[u3]
                TT(q_[:, 10:12], q_[:, 6:8], q_[:, 8:10], ALU.add, [bst4[u3]], [bst4[u3]])
                dve.op(lambda e, o_=q_[:, 12:14], i_=q_[:, 10:12]: e.reciprocal(out=o_, in_=i_), [bst4[u3]], [bst4[u3]])

            def sw_U2b(n):
                u = n % 2
                for h in range(2):
                    for half in range(2):
                        c = (h * 2 + half) * 128
                        TR(PSB[7][:, c:c + 128], pex[u][:, h * 256 + half * 128:h * 256 + (half + 1) * 128], identb, [bpex[u], bconst], [bPS[7]],
                           sig=(h == 1 and half == 1))
                ACT(pT[u], PSB[7][:, 0:512], AF.Copy, [bPS[7]], [bpT[u]])

            def sw_U3(n):
                i, m, j = units[n]
                u, u3 = n % 2, n % 3
                q_ = st4[u3]
                s_ = i % 2
                bank = 2 + u
                for h in range(2):
                    for half in range(2):
                        c = (h * 2 + half) * 128
                        MM(PS[bank][:, h * 64:(h + 1) * 64], pT[u][:, c:c + 128], vsw[:, m + half, h * 64:(h + 1) * 64], half == 0, half == 1,
                           [bpT[u], bvsw], [bPS[bank]], sig=(h == 1 and half == 1))
                for h in range(2):
                    head = j + 4 * h
                    TS(ost[s_][:, m, head * 64:(head + 1) * 64], PS[bank][:, h * 64:(h + 1) * 64], q_[:, 12 + h:13 + h], None, ALU.mult, None,
                       [bPS[bank], bst4[u3]], [bost[s_]], partial=True)
                if m == 3 and j == 3:
                    pool.dma(O_s[i * 512:(i + 1) * 512, 512:1024].rearrange("(a p) c -> p a c", p=128), ost[s_], "st_v%d" % s_, reads=[bost[s_]], writes=[bO], partial=True)

            def p1b_B(i):
                s_ = i % 2
                H = hT[s_]
                cT_, sT_ = cosT[s_], sinT[s_]
                for hp in range(4):
                    bank = 2 + hp % 2
                    for k in range(8):
                        MM(PS[bank][:, :], win[:, k, C_Q + hp * 128:C_Q + (hp + 1) * 128], H[:, k, 128:640], k == 0, k == 7,
                           [bwin, bhT[s_]], [bPS[bank]], sig=(k == 7))
                    if hp % 2 == 0:
                        ACT(qst[s_][:, hp, :], PS[bank][:, :], AF.Identity, [bPS[bank]], [bqst[s_]], scale=0.125, partial=(hp > 0))
                    else:
                        TS(qst[s_][:, hp, :], PS[bank][:, :], 0.125, None, ALU.mult, None, [bPS[bank]], [bqst[s_]], partial=True)
                pool.dma(QT_s[:, :, i * 512:(i + 1) * 512].rearrange("h p t -> p h t"), qst[s_], "st_k%d" % s_, reads=[bqst[s_]], writes=[bQT], partial=True)
                for (a0, a1) in ((0, 128), (128, 640)):
                    w_ = a1 - a0
                    for k in range(8):
                        MM(PS[4][:, 0:w_], win[:, k, C_KS:C_KS + 128], H[:, k, a0:a1], k == 0, k == 7, [bwin, bhT[s_]], [bPS[4]], sig=(k == 7))
                    for k in range(8):
                        MM(PS[5][:, 0:w_], win[:, k, C_KSS:C_KSS + 128], H[:, k, a0:a1], k == 0, k == 7, [bwin, bhT[s_]], [bPS[5]], sig=(k == 7))
                    TT(t1[:, 0:w_], PS[4][:, 0:w_], cT_[:, a0:a1], ALU.mult, [bPS[4], bcs[s_]], [bt1])
                    TT(t2[:, 0:w_], PS[5][:, 0:w_], sT_[:, a0:a1], ALU.mult, [bPS[5], bcs[s_]], [bt2])
                    TT(ksT[:, a0:a1], t1[:, 0:w_], t2[:, 0:w_], ALU.add, [bt1, bt2], [bksT], partial=(a0 > 0))
                for j in range(4):
                    for k in range(8):
                        MM(PS[4][:, :], win[:, k, C_QS + j * 128:C_QS + (j + 1) * 128], H[:, k, 128:640], k == 0, k == 7, [bwin, bhT[s_]], [bPS[4]], sig=(k == 7))
                    for k in range(8):
                        MM(PS[5][:, :], win[:, k, C_QSS + j * 128:C_QSS + (j + 1) * 128], H[:, k, 128:640], k == 0, k == 7, [bwin, bhT[s_]], [bPS[5]], sig=(k == 7))
                    TT(t1[:, 0:512], PS[4][:, :], cT_[:, 128:640], ALU.mult, [bPS[4], bcs[s_]], [bt1])
                    TT(t2[:, 0:512], PS[5][:, :], sT_[:, 128:640], ALU.mult, [bPS[5], bcs[s_]], [bt2])
                    TT(qsT[:, j, :], t1[:, 0:512], t2[:, 0:512], ALU.add, [bt1, bt2], [bqsT], partial=(j > 0))
                for a in range(5):
                    bank = 2 + a % 2
                    for k in range(8):
                        MM(PS[bank][:, 0:128], H[:, k, a * 128:(a + 1) * 128], win[:, k, C_VS:C_VS + 128], k == 0, k == 7, [bwin, bhT[s_]], [bPS[bank]], sig=(k == 7))
                    CP(vsw[:, a, :], PS[bank][:, 0:128], [bPS[bank]], [bvsw], partial=(a > 0))
                n0 = len(units)
                for m in range(4):
                    for j in range(4):
                        units.append((i, m, j))
                n1 = len(units)
                for it in range(n0, n1 + 3):
                    if n0 <= it - 3 < n1:
                        sw_U3(it - 3)
                    if n0 <= it - 2 < n1:
                        sw_U2b(it - 2)
                        sw_U2d(it - 2)
                    if n0 <= it - 1 < n1:
                        sw_U2(it - 1)
                    if it < n1:
                        sw_U1(it)

            sp.dma(xt[0], xo_v[0], "ld_x0", writes=[bxt[0]])
            sp.dma(xt[1], xo_v[1], "ld_x1", writes=[bxt[1]])
            sp.dma(pos_i, posb[0], "ld_pos", writes=[bpos])
            for it in range(8 + 3):
                if 0 <= it - 3 < 8:
                    p1b_B(it - 3)
                if 0 <= it - 2 < 8:
                    nt_T3(5, (it - 2) % 2)
                if 0 <= it - 1 < 8:
                    i_ = it - 1
                    nt_T2(5, i_ % 2, i_ % 3)
                    if i_ + 2 < 8:
                        sp.dma(xt[i_ % 2], xo_v[i_ + 2], "ld_x%d" % (i_ % 2), writes=[bxt[i_ % 2]])
                    p1b_R(i_)
                if it < 8:
                    nt_T1(5, it % 2, it % 3)
            fw.barrier()

        if stop_after >= 3:
            A.reset(PERSIST_END)
            KTp = [A.get(BF16, [128, S]) for _ in range(2)]
            Vp = [A.get(BF16, [128, 64, 128]) for _ in range(2)]
            QTp = [A.get(BF16, [128, 4096]) for _ in range(2)]
            amk = A.get(BF16, [128, 16, 512])
            e_t = [A.get(F32, [128, 1024]) for _ in range(2)]
            l_t = [A.get(BF16, [128, 1024]) for _ in range(2)]
            lsum = A.get(F32, [128, 512])
            lsb = [A.get(BF16, [128, 2, 512]) for _ in range(3)]
            a_t = [A.get(BF16, [128, 1024]) for _ in range(2)]
            osb = [A.get(BF16, [128, 4, 128]) for _ in range(2)]
            bKTp, bVp, bQTp = [Buf("KTp0"), Buf("KTp1")], [Buf("Vp0"), Buf("Vp1")], [Buf("QTp0"), Buf("QTp1")]
            bamk = Buf("amk")
            be, bl, blsum, ba, bosb = [Buf("e0"), Buf("e1")], [Buf("l0"), Buf("l1")], Buf("lsum"), [Buf("a0"), Buf("a1")], [Buf("osb0"), Buf("osb1")]
            blsb = [[Buf("lsb%d%d" % (x_, y_)) for y_ in range(2)] for x_ in range(3)]
            bPQ = [Buf("pq%d" % x_) for x_ in range(3)]
            for g in range(4):
                pool.dma(amk[:, g * 4:(g + 1) * 4, :], amask[g * 4:(g + 1) * 4].rearrange("j p c -> p j c"), "ld_amk", writes=[bamk], partial=True)

            def load_pair(hp, s_):
                sp.dma(KTp[s_], KT_s[hp], "ld_kt%d" % s_, reads=[bKT], writes=[bKTp[s_]])
                for g in range(4):
                    sp.dma(Vp[s_][:, g * 16:(g + 1) * 16, :], V_s[g * 2048:(g + 1) * 2048, hp * 128:(hp + 1) * 128].rearrange("(kb p) c -> p kb c", p=128),
                           "ld_v%d" % s_, reads=[bV], writes=[bVp[s_]], partial=(g > 0))
                sp.dma(QTp[s_], QT_s[hp], "ld_qt%d" % s_, reads=[bQT], writes=[bQTp[s_]])

            steps = []
            tid = 0
            for hp in range(4):
                for i in range(8):
                    for hh in range(2):
                        nT = (8 * i + 8) // 2
                        for tl in range(nT):
                            kbA = 8 * i + 7 - 2 * tl
                            steps.append(dict(hp=hp, s=hp % 2, i=i, hh=hh, tl=tl, nT=nT, kb=(kbA, kbA - 1),
                                              tid=tid, osl=(hp * 8 + i) % 2, pfirst=(i == 0 and hh == 0 and tl == 0)))
                        tid += 1
            NS = len(steps)

            def stage1(t):
                d = steps[t]
                u, p3 = t % 2, t % 3
                s_ = d["s"]
                pq = PSP[p3]
                pr = slice(d["hh"] * 64, (d["hh"] + 1) * 64)
                q_ap = QTp[s_][pr, d["i"] * 512:(d["i"] + 1) * 512]
                for half in range(2):
                    kb = d["kb"][half]
                    masked = kb >= 8 * d["i"]
                    dst = pq[:, half * 512:(half + 1) * 512]
                    MM(dst, KTp[s_][pr, kb * 128:(kb + 1) * 128], q_ap, True, not masked, [bKTp[s_], bQTp[s_]], [bPQ[p3]], sig=(not masked))
                    if masked:
                        MM(dst, identb, amk[:, (d["i"] % 2) * 8 + (kb - 8 * d["i"]), :], False, True, [bconst, bamk], [bPQ[p3]], sig=True)
                ACT(e_t[u], pq[:, :], AF.Exp, [bPQ[p3]], [be[u]])
                ACT(l_t[u], e_t[u], AF.Ln, [be[u]], [bl[u]], bias=1.0)
                if d["tl"] == 0:
                    CP(lsum, l_t[u][:, 0:512], [bl[u]], [blsum])
                else:
                    TT(lsum, lsum, l_t[u][:, 0:512], ALU.add, [blsum, bl[u]], [blsum])
                CP(lsb[p3][:, 0, :], lsum, [blsum], [blsb[p3][0]])
                if d["tl"] + 1 < d["nT"]:
                    TT(lsum, lsum, l_t[u][:, 512:1024], ALU.add, [blsum, bl[u]], [blsum])
                    CP(lsb[p3][:, 1, :], lsum, [blsum], [blsb[p3][1]])

            def stage2(t):
                d = steps[t]
                u, p3 = t % 2, t % 3
                pq = PSP[p3]
                pm = (t - 1) % 3
                MM(pq[:, 0:512], trin, l_t[u][:, 0:512], False, d["tl"] == 0, [bconst, bl[u]], [bPQ[p3]], sig=False, skip=True)
                if d["tl"] > 0:
                    MM(pq[:, 0:512], onesn, lsb[pm][:, 1, :], False, True, [bconst, blsb[pm][1]], [bPQ[p3]], sig=False, skip=True)
                MM(pq[:, 512:1024], trin, l_t[u][:, 512:1024], False, False, [bconst, bl[u]], [bPQ[p3]], sig=False, skip=True)
                MM(pq[:, 512:1024], onesn, lsb[p3][:, 0, :], False, True, [bconst, blsb[p3][0]], [bPQ[p3]], sig=True, skip=True)
                ACT(a_t[u], pq[:, :], AF.Exp, [bPQ[p3]], [ba[u]])

            def stage3(t):
                d = steps[t]
                u = t % 2
                s_ = d["s"]
                ou = d["tid"] % 2
                pso, bpso = PS[6 + ou], bPS[6 + ou]
                hh, osl = d["hh"], d["osl"]
                last = d["tl"] == d["nT"] - 1
                for half in range(2):
                    kb = d["kb"][half]
                    for qb in range(4):
                        MM(pso[:, qb * 64:(qb + 1) * 64], a_t[u][:, half * 512 + qb * 128:half * 512 + (qb + 1) * 128], Vp[s_][:, kb, hh * 64:(hh + 1) * 64],
                           (d["tl"] == 0 and half == 0 and qb == 0), (last and half == 1), [ba[u], bVp[s_]], [bpso], sig=(half == 1 and qb == 3), skip=True)
                if last:
                    CP(osb[osl][:, :, hh * 64:(hh + 1) * 64], pso[:, 0:256].rearrange("p (a d) -> p a d", d=64), [bpso], [bosb[osl]], partial=(hh > 0))
                    if hh == 1:
                        i, hp = d["i"], d["hp"]
                        sp.dma(O_s[i * 512:(i + 1) * 512, hp * 128:(hp + 1) * 128].rearrange("(a p) c -> p a c", p=128), osb[osl], "st_o%d" % osl,
                               reads=[bosb[osl]], writes=[bO], partial=True)

            load_pair(0, 0)
            pending_load = None
            for it in range(NS + 2):
                if it < NS:
                    d = steps[it]
                    if d["pfirst"] and d["hp"] + 1 < 4:
                        pending_load = (it + 2, d["hp"] + 1)
                    stage1(it)
                if 0 <= it - 1 < NS:
                    stage2(it - 1)
                if 0 <= it - 2 < NS:
                    stage3(it - 2)
                if pending_load is not None and it >= pending_load[0]:
                    load_pair(pending_load[1], pending_load[1] % 2)
                    pending_load = None
            fw.barrier()

        if stop_after >= 4:
            A.reset(PERSIST_END)
            wout = A.get(BF16, [128, 8, D])
            wr = A.get(F32, [128, 8, 20])
            ob = [A.get(BF16, [128, D]) for _ in range(2)]
            on = [A.get(BF16, [128, D]) for _ in range(2)]
            mixT = [A.get(BF16, [128, 8, 128]) for _ in range(2)]
            xb = [A.get(F32, [128, D]) for _ in range(2)]
            x1 = [A.get(F32, [128, D]) for _ in range(3)]
            xs2 = [A.get(F32, [128, D]) for _ in range(2)]
            s4x = [A.get(F32, [128, 8]) for _ in range(3)]
            h2fs = [A.get(F32, [128, 8, 128]) for _ in range(2)]
            h2b = [A.get(BF16, [128, 8, 128]) for _ in range(2)]
            junk4 = A.get(F32, [128, D])
            s4 = [A.get(F32, [128, 8]) for _ in range(3)]
            rl = [A.get(F32, [128, 32]) for _ in range(2)]
            rt = [A.get(F32, [128, 64]) for _ in range(3)]
            bwout, bwr = Buf("wout"), Buf("wr")
            bob, bon, bmixT = [Buf("ob0"), Buf("ob1")], [Buf("on0"), Buf("on1")], [Buf("mixT0"), Buf("mixT1")]
            bxb, bx1, bxs2, bh2f, bh2b = [Buf("xb0"), Buf("xb1")], [Buf("x10"), Buf("x11"), Buf("x12")], [Buf("xs20"), Buf("xs21")], None, [Buf("h2b0"), Buf("h2b1")]
            bs4x = [Buf("s4x0"), Buf("s4x1"), Buf("s4x2")]
            bh2fs = [Buf("h2f0"), Buf("h2f1")]
            bj4, bs4, brl, brt = Buf("junk4"), [Buf("s40"), Buf("s41"), Buf("s42")], [Buf("rl0"), Buf("rl1")], [Buf("rt0"), Buf("rt1"), Buf("rt2")]
            for k in range(8):
                pool.dma(wout[:, k, :], w_out[k * 128:(k + 1) * 128, :], "ld_wout", writes=[bwout], partial=True)
            sp.dma(wr, w_r.rearrange("(k p) n -> p k n", p=128), "ld_wr", writes=[bwr])

            def p4_load(blk, s_):
                sp.dma(ob[s_], O_s[blk * 128:(blk + 1) * 128, :], "ld_ob%d" % s_, reads=[bO], writes=[bob[s_]])

            def p4_load_x(blk):
                i, m = blk // 4, blk % 4
                s_ = blk % 2
                sp.dma(xb[s_], xo[i, 128 + m * 128:256 + m * 128, :], "ld_xb%d" % s_, writes=[bxb[s_]])

            def st_A(blk):
                s_ = blk % 2
                q_ = s4[blk % 3]
                for h in range(2):
                    ACT(on[s_][:, h * 512:(h + 1) * 512], ob[s_][:, h * 512:(h + 1) * 512], AF.Square, [bob[s_]], [bon[s_], bs4[blk % 3]], accum=q_[:, h:h + 1], partial=True)
                rstd_from_ssq(q_[:, 2:4], q_[:, 0:2], 512.0, [bs4[blk % 3]], [bs4[blk % 3]])

            def st_B(blk):
                s_ = blk % 2
                q_ = s4[blk % 3]
                for h in range(2):
                    TS(on[s_][:, h * 512:(h + 1) * 512], ob[s_][:, h * 512:(h + 1) * 512], q_[:, 2 + h:3 + h], None, ALU.mult, None, [bob[s_], bs4[blk % 3]], [bon[s_]], partial=(h > 0))
                if blk + 2 < 32:
                    p4_load(blk + 2, s_)

            def st_C(blk):
                s_ = blk % 2
                for k in range(8):
                    TR(PSB[0][:, k * 128:(k + 1) * 128], on[s_][:, k * 128:(k + 1) * 128], identb, [bon[s_], bconst], [bPS[0]], sig=(k == 7))
                TT(mixT[s_], PSB[0][:, :].rearrange("p (k t) -> p k t", t=128), G_o, ALU.mult, [bPS[0], bgates], [bmixT[s_]])

            def st_D(blk):
                s_ = blk % 2
                X1 = x1[blk % 3]
                bX = bx1[blk % 3]
                for nt in range(2):
                    for k in range(8):
                        MM(PS[1 + nt][:, :], mixT[s_][:, k, :], wout[:, k, nt * 512:(nt + 1) * 512], k == 0, k == 7, [bmixT[s_], bwout], [bPS[1 + nt]], sig=(k == 7))
                    TT(X1[:, nt * 512:(nt + 1) * 512], PS[1 + nt][:, :], gate1_t[:, nt * 512:(nt + 1) * 512], ALU.mult, [bPS[1 + nt], bgates], [bX], partial=(nt > 0))
                TT(X1, X1, xb[s_], ALU.add, [bX, bxb[s_]], [bX])
                sp.dma(X1_s[blk * 128:(blk + 1) * 128, :], X1, "st_x1%d" % (blk % 3), reads=[bX], writes=[bX1], partial=True)

            def st_E(blk):
                q_ = s4x[blk % 3]
                ACT(junk4, x1[blk % 3], AF.Square, [bx1[blk % 3]], [bj4, bs4x[blk % 3]], accum=q_[:, 0:1], partial=False)
                rstd_from_ssq(q_[:, 1:2], q_[:, 0:1], float(D), [bs4x[blk % 3]], [bs4x[blk % 3]])

            def st_F(blk):
                s_ = blk % 2
                q_ = s4x[blk % 3]
                TS(xs2[s_], x1[blk % 3], q_[:, 1:2], None, ALU.mult, None, [bx1[blk % 3], bs4x[blk % 3]], [bxs2[s_]])

            def st_G(blk):
                s_ = blk % 2
                h2f = h2fs[s_]
                bh2f = bh2fs[s_]
                for half in range(2):
                    bank = 3 + half
                    for kk in range(4):
                        k = half * 4 + kk
                        TR(PS[bank][:, kk * 128:(kk + 1) * 128], xs2[s_][:, k * 128:(k + 1) * 128], identf, [bxs2[s_], bconst], [bPS[bank]], sig=(kk == 3))
                    for kk in range(4):
                        k = half * 4 + kk
                        if half == 0:
                            ACT(h2f[:, k, :], PS[bank][:, kk * 128:(kk + 1) * 128], AF.Identity, [bPS[bank], bmod], [bh2f], bias=sh2[:, k:k + 1], scale=gm2[:, k:k + 1], partial=(k > 0))
                        else:
                            TS(h2f[:, k, :], PS[bank][:, kk * 128:(kk + 1) * 128], gm2[:, k:k + 1], sh2[:, k:k + 1], ALU.mult, ALU.add, [bPS[bank], bmod], [bh2f], partial=True)
                CP(h2b[s_], h2f, [bh2f], [bh2b[s_]])
                sp.dma(H2T_s[:, :, blk * 128:(blk + 1) * 128].rearrange("k p t -> p k t"), h2b[s_], "st_h2%d" % s_, reads=[bh2b[s_]], writes=[bH2T], partial=True)

            def st_H(blk):
                s_ = blk % 2
                h2f = h2fs[s_]
                bh2f = bh2fs[s_]
                for k in range(8):
                    MM(PS[5][:, 0:20], h2f[:, k, :], wr[:, k, :], k == 0, k == 7, [bh2f, bwr], [bPS[5]], sig=(k == 7))
                CP(rl[s_][:, 0:20], PS[5][:, 0:20], [bPS[5]], [brl[s_]])

            def st_I(blk):
                s_ = blk % 2
                R, bR = rt[blk % 3], brt[blk % 3]
                L_, bL = rl[s_], brl[s_]
                dve.op(lambda e, o_=R[:, 0:1], i_=L_[:, 0:4]: e.tensor_reduce(out=o_, in_=i_, axis=AX.X, op=ALU.max), [bL], [bR])
                TS(R[:, 1:2], R[:, 0:1], -1.0, None, ALU.mult, None, [bR], [bR])
                TS(R[:, 8:12], L_[:, 0:4], R[:, 0:1], None, ALU.is_ge, None, [bL, bR], [bR])
                TS(R[:, 12:16], R[:, 8:12], BIG, -BIG, ALU.mult, ALU.add, [bR], [bR])
                for g in range(4):
                    TS(R[:, 16 + 4 * g:20 + 4 * g], L_[:, 4 + 4 * g:8 + 4 * g], R[:, 8 + g:9 + g], R[:, 12 + g:13 + g], ALU.mult, ALU.add, [bL, bR], [bR])
                dve.op(lambda e, o_=R[:, 32:40], i_=R[:, 16:32]: e.max(out=o_, in_=i_), [bR], [bR])
                TS(R[:, 40:56], R[:, 16:32], R[:, 33:34], None, ALU.is_ge, None, [bR], [bR])
                TS(R[:, 3:4], R[:, 32:33], -1.0, None, ALU.mult, None, [bR], [bR])
                TT(R[:, 56:57], R[:, 33:34], R[:, 32:33], ALU.subtract, [bR], [bR])
                CP(R[:, 60:64], L_[:, 0:4], [bL, bR], [bR])

            def st_J(blk):
                R, bR = rt[blk % 3], brt[blk % 3]
                ACT(R[:, 4:8], R[:, 60:64], AF.Exp, [bR], [bR], bias=R[:, 1:2], accum=R[:, 2:3])
                ACT(R[:, 16:32], R[:, 16:32], AF.Exp, [bR], [bR], bias=R[:, 3:4])
                ACT(R[:, 57:58], R[:, 56:57], AF.Exp, [bR], [bR])

            def st_K(blk):
                R, bR = rt[blk % 3], brt[blk % 3]
                TS(R[:, 58:59], R[:, 57:58], 1.0, R[:, 2:3], ALU.add, ALU.mult, [bR], [bR])
                dve.op(lambda e, o_=R[:, 59:60], i_=R[:, 58:59]: e.reciprocal(out=o_, in_=i_), [bR], [bR])
                STT(GW[:, blk, :], R[:, 16:32], R[:, 59:60], R[:, 40:56], ALU.mult, ALU.mult, [bR], [bGW], partial=True)

            stages = [st_A, st_B, st_C, st_D, st_E, st_F, st_G, st_H, st_I, st_J, st_K]
            p4_load(0, 0)
            p4_load(1, 1)
            p4_load_x(0)
            p4_load_x(1)
            for it in range(32 + len(stages) - 1):
                for j in range(len(stages) - 1, -1, -1):
                    blk = it - j
                    if 0 <= blk < 32:
                        stages[j](blk)
                        if j == 3 and blk + 2 < 32:
                            p4_load_x(blk + 2)
            if "GW_s" in dbg:
                sp.dma(GW_s, GW.rearrange("p a b -> p (a b)"), "st_dbg", reads=[bGW], writes=[bDBG], partial=True)
            fw.barrier()

        if stop_after >= 5:
            A.reset(PERSIST_END)
            acc = A.get(F32, [128, 16, D])
            h2T = A.get(BF16, [128, 8, 2048])
            wg = [A.get(BF16, [128, 8, 512]) for _ in range(2)]
            wu = [A.get(BF16, [128, 8, 512]) for _ in range(2)]
            wd = [A.get(BF16, [128, 4, D]) for _ in range(2)]
            wds = A.get(F32, [128, 4, D])
            hid = [A.get(BF16, [128, 4, 512]) for _ in range(2)]
            sg = [A.get(F32, [128, 512]) for _ in range(2)]
            fo = [A.get(F32, [128, D]) for _ in range(2)]
            s5 = [A.get(F32, [128, 4]) for _ in range(2)]
            junk5 = sg[0]
            bacc = [Buf("acc%d" % b_) for b_ in range(16)]
            bwg, bwu, bwd, bwds = [Buf("wg0"), Buf("wg1")], [Buf("wu0"), Buf("wu1")], [Buf("wd0"), Buf("wd1")], Buf("wds")
            bhid, bsg, bfo, bs5 = [Buf("hid0"), Buf("hid1")], [Buf("sg0"), Buf("sg1")], [Buf("fo0"), Buf("fo1")], [Buf("s50"), Buf("s51")]

            def load_w(e_, s_):
                pool.dma(wg[s_], w_gate[e_].rearrange("(k p) n -> p k n", p=128), "ld_wg%d" % s_, writes=[bwg[s_]])
                pool.dma(wu[s_], w_up[e_].rearrange("(k p) n -> p k n", p=128), "ld_wu%d" % s_, writes=[bwu[s_]])
                sp.dma(wds, w_down[e_].rearrange("(m p) f -> p m f", p=128), "ld_wds", writes=[bwds])
                for m in range(4):
                    TT(wd[s_][:, m, :], wds[:, m, :], gate2_t, ALU.mult, [bwds, bgates], [bwd[s_]], partial=(m > 0), eng=pool)

            gcnt = 0
            ycnt = 0
            wcnt = 0
            bh2Tt = [Buf("h2T%d" % x_) for x_ in range(4)]

            def load_acc(sb, g):
                sp.dma(acc[:, g * 4:(g + 1) * 4, :], X1_s[sb * 2048 + g * 512:sb * 2048 + (g + 1) * 512, :].rearrange("(a p) f -> p a f", p=128),
                       "ld_acc%d" % g, reads=[bX1], writes=[bacc[g * 4 + a] for a in range(4)])

            def load_h2T(sb, tt_):
                sp.dma(h2T[:, :, tt_ * 512:(tt_ + 1) * 512], H2T_s[:, :, sb * 2048 + tt_ * 512:sb * 2048 + (tt_ + 1) * 512].rearrange("k p t -> p k t"),
                       "ld_h2T%d" % tt_, reads=[bH2T], writes=[bh2Tt[tt_]])

            def final_norm(sb, blk):
                f_ = blk % 2
                q_ = s5[f_]
                ACT(fo[f_], acc[:, blk, :], AF.Square, [bacc[blk]], [bfo[f_], bs5[f_]], accum=q_[:, 0:1])
                rstd_from_ssq(q_[:, 1:2], q_[:, 0:1], float(D), [bs5[f_]], [bs5[f_]])
                STT(fo[f_], acc[:, blk, :], q_[:, 1:2], gfin_t, ALU.mult, ALU.mult, [bacc[blk], bs5[f_], bgates], [bfo[f_]])
                sp.dma(out[sb * 2048 + blk * 128:sb * 2048 + (blk + 1) * 128, :], fo[f_], "st_out%d" % f_, reads=[bfo[f_]], writes=[bOUT], partial=True)

            jobs = [(sb, e_, tt_) for sb in range(2) for e_ in range(NEXP) for tt_ in range(4)]
            NJ = len(jobs)

            def wslot(sb, e_):
                return (sb * NEXP + e_) % 2

            def job_GU(g):
                sb, e_, tt_ = jobs[g]
                ws = wslot(sb, e_)
                hs = g % 2
                tok = slice(tt_ * 512, (tt_ + 1) * 512)
                for m in range(4):
                    gu = (g * 4 + m) % 2
                    for k in range(8):
                        MM(PS[gu][:, :], wg[ws][:, k, m * 128:(m + 1) * 128], h2T[:, k, tok], k == 0, k == 7, [bwg[ws], bh2Tt[tt_]], [bPS[gu]], sig=(k == 7))
                    for k in range(8):
                        MM(PS[2 + gu][:, :], wu[ws][:, k, m * 128:(m + 1) * 128], h2T[:, k, tok], k == 0, k == 7, [bwu[ws], bh2Tt[tt_]], [bPS[2 + gu]], sig=(k == 7))
                    ACT(sg[gu], PS[gu][:, :], AF.Silu, [bPS[gu]], [bsg[gu]])
                    TT(hid[hs][:, m, :], sg[gu], PS[2 + gu][:, :], ALU.mult, [bsg[gu], bPS[2 + gu]], [bhid[hs]], partial=(m > 0))
                if e_ == NEXP - 1 and sb == 0:
                    load_h2T(1, tt_)

            def job_D(g):
                sb, e_, tt_ = jobs[g]
                ws = wslot(sb, e_)
                hs = g % 2
                for a in range(4):
                    blk = tt_ * 4 + a
                    gblk = sb * 16 + blk
                    for nt in range(2):
                        yb = 4 + (g * 8 + a * 2 + nt) % 4
                        for m in range(4):
                            MM(PS[yb][:, :], hid[hs][:, m, a * 128:(a + 1) * 128], wd[ws][:, m, nt * 512:(nt + 1) * 512], m == 0, m == 3,
                               [bhid[hs], bwd[ws]], [bPS[yb]], sig=(m == 3))
                        STT(acc[:, blk, nt * 512:(nt + 1) * 512], PS[yb][:, :], GW[:, gblk, e_:e_ + 1], acc[:, blk, nt * 512:(nt + 1) * 512], ALU.mult, ALU.add,
                            [bPS[yb], bGW, bacc[blk]], [bacc[blk]], partial=True)
                if e_ == NEXP - 1:
                    for a in range(4):
                        final_norm(sb, tt_ * 4 + a)
                    if sb == 0:
                        load_acc(1, tt_)
                if tt_ == 3:
                    nxt = sb * NEXP + e_ + 2
                    if nxt < 2 * NEXP:
                        load_w(nxt % NEXP, ws)

            for g in range(4):
                load_h2T(0, g)
            for g in range(4):
                load_acc(0, g)
            load_w(0, 0)
            load_w(1, 1)
            job_GU(0)
            for g in range(NJ):
                if g + 1 < NJ:
                    job_GU(g + 1)
                job_D(g)

        for q in (sp,):
            q.wait_all([bOUT, bDBG, bKT, bV, bQT, bO, bX1, bH2T])
        fw.finish()
        stats = {e.name: e.n_instr for e in fw.engs}
    return nc, stats


def _consts():
    j = np.arange(128)
    ident = np.eye(128, dtype=np.float32)
    trin = np.where(j[:, None] >= j[None, :], -1.0, 0.0).astype(np.float32)
    onesn = -np.ones((128, 128), np.float32)
    ones = np.ones((128, 128), np.float32)
    return np.stack([ident, trin, onesn, ones])


def _amask(r):
    out = np.zeros((2, 8, 128, 512), np.float32)
    sl = np.arange(128)[:, None]
    tl = np.arange(512)[None, :]
    for par in range(2):
        v = par if r == 0 else 1 - par
        for jj in range(8):
            valid = (128 * jj + sl) < (512 * v + tl)
            out[par, jj] = np.where(valid, 0.0, NEG)
    return out.reshape(16, 128, 512)


def _swb(r):
    qi = np.arange(128)[:, None]
    kk = np.arange(256)[None, :]
    diff = 128 + qi - kk
    ok = (diff >= 0) & (diff < 128)
    norm = np.where(ok, 0.0, NEG).astype(np.float32)
    first = np.where(ok & (kk >= 128), 0.0, NEG).astype(np.float32) if r == 0 else norm
    return np.stack([np.concatenate([norm, norm], 1), np.concatenate([first, first], 1)])


def _prep_inputs(x, c, positions, w_ada, b_ada, norm_mix_g, w_in, sinks, out_norm_sb_g, out_norm_sw_g,
                 w_out, norm_ffn_g, w_router_group, w_router_expert, w_gate, w_up, w_down, norm_final_g):
    f32 = np.float32
    x = np.asarray(x, f32)
    positions = np.asarray(positions, np.int32)
    w_in0 = np.asarray(w_in[0], f32)

    def chunkT(v):
        return np.ascontiguousarray(np.asarray(v, f32).reshape(-1, 128).T)

    qs = w_in0[:, 1536:2048].reshape(D, 8, 64)
    qs_perm = np.concatenate([np.concatenate([qs[:, j], qs[:, 4 + j]], axis=1) for j in range(4)], axis=1)
    qs_sw = np.concatenate([qs[:, :, 32:], qs[:, :, :32]], axis=2)
    qs_sw_perm = np.concatenate([np.concatenate([qs_sw[:, j], qs_sw[:, 4 + j]], axis=1) for j in range(4)], axis=1)
    ks = w_in0[:, 2048:2176].reshape(D, 2, 64)
    ks_sw = np.concatenate([ks[:, :, 32:], ks[:, :, :32]], axis=2).reshape(D, 128)
    w_inr = np.ascontiguousarray(np.concatenate([
        w_in0[:, 0:512], w_in0[:, 512:1024], w_in0[:, 1024:1536], qs_perm, qs_sw_perm,
        w_in0[:, 2048:2176], ks_sw, w_in0[:, 2176:2304]], axis=1))
    assert w_inr.shape[1] == C_END
    sk = np.asarray(sinks[0], f32)
    sink_cols = np.stack([sk[[j, 4 + j]] for j in range(4)]).reshape(8)
    inv_freq = (10000.0 ** (-np.arange(0, 64, 2, dtype=np.float32) / 64)).astype(f32)
    p = np.arange(128)
    invf = inv_freq[p % 32]
    sgn = np.where((p % 64) < 32, -1.0, 1.0).astype(f32)
    g_o = np.concatenate([np.asarray(out_norm_sb_g[0], f32), np.asarray(out_norm_sw_g[0], f32)])
    cm = _consts()
    w_r = np.ascontiguousarray(np.concatenate([np.asarray(w_router_group[0], f32), np.asarray(w_router_expert[0], f32)], axis=1))
    shared = dict(w_ada=np.ascontiguousarray(np.asarray(w_ada[0], f32)), w_inr=w_inr,
                  w_out=np.ascontiguousarray(np.asarray(w_out[0], f32)), w_r=w_r,
                  w_gate=np.ascontiguousarray(np.asarray(w_gate[0], f32)), w_up=np.ascontiguousarray(np.asarray(w_up[0], f32)),
                  w_down=np.ascontiguousarray(np.asarray(w_down[0], f32)), cmat=cm)
    in_maps = []
    for core in range(8):
        b, r = core // 2, core % 2
        xo = np.zeros((8, 640, D), f32)
        po = np.zeros((8, 640), np.int32)
        for i, qt in enumerate(T_R[r]):
            lo = 512 * qt - 128
            if lo < 0:
                xo[i, 128:] = x[b, 0:512]
                po[i, 128:] = positions[b, 0:512]
            else:
                xo[i] = x[b, lo:lo + 640]
                po[i] = positions[b, lo:lo + 640]
        vecs = np.zeros((128, V_N), f32)
        vecs[:, V_BADA:V_BADA + 48] = np.asarray(b_ada[0], f32).reshape(48, 128).T
        vecs[:, V_GMIX:V_GMIX + 8] = chunkT(norm_mix_g[0])
        vecs[:, V_GFFN:V_GFFN + 8] = chunkT(norm_ffn_g[0])
        vecs[:, V_GO:V_GO + 8] = chunkT(g_o)
        vecs[:, V_GFIN:V_GFIN + 8] = chunkT(norm_final_g)
        vecs[:, V_SINK:V_SINK + 8] = sink_cols[None, :]
        vecs[:, V_INVF] = invf
        vecs[:, V_SGN] = sgn
        m = dict(shared)
        m.update(xs=np.ascontiguousarray(x[b]), xo=xo,
                 posb=np.ascontiguousarray(np.broadcast_to(po[:, None, :], (8, 128, 640))),
                 cT=chunkT(np.asarray(c, f32)[b]), vecs=vecs, amask=_amask(r), swb=_swb(r))
        in_maps.append(m)
    return in_maps


_CACHE = {}


def kernel(**inputs):
    in_maps = _prep_inputs(**inputs)
    if "nc" not in _CACHE:
        _CACHE["nc"] = build_program()[0]
    nc = _CACHE["nc"]
    res = run_bass_kernel_spmd(nc, in_maps, core_ids=list(range(8)))
    out = np.zeros((4, S, D), np.float32)
    for core in range(8):
        b, r = core // 2, core % 2
        o = np.asarray(res.results[core]["out"], np.float32).reshape(8, 512, D)
        for i, qt in enumerate(T_R[r]):
            out[b, 512 * qt:512 * (qt + 1)] = o[i]
    return out
```
